# Optimizing a Trainium2 kernel written in Bass

```python
import jax, jax.numpy as jnp
from jax import lax
import numpy as np

D_MODEL = 1024
BATCH = 8
SEQ = 4096
DEPTH = 2

MIX_WIDTH = D_MODEL
MLA_HEADS = 8
MLA_V = (MIX_WIDTH // 2) // MLA_HEADS
MLA_NOPE = 64
MLA_ROPE = 32
MLA_Q_LORA = D_MODEL // 4
MLA_KV_LORA = D_MODEL // 8
FOX_HEADS = 8
FOX_HEAD_DIM = (MIX_WIDTH // 2) // FOX_HEADS
FOX_W = FOX_HEADS * FOX_HEAD_DIM
IN_SPLITS = (MLA_Q_LORA, MLA_KV_LORA, MLA_ROPE, FOX_W, FOX_W, FOX_W, FOX_HEADS)
IN_COLS = sum(IN_SPLITS)
Q_BLOCK = 128
ROPE_THETA = 10000.0
D_FF = D_MODEL * 7 // 2
N_EXPERTS = 8
TOP_K = 2
EPS = 1e-6

kernel_name = "hybrid_mla_fox_adaln_moe_trunk"


def rms_norm(x, g):
    xf = x.astype(jnp.float32)
    y = xf * lax.rsqrt(jnp.mean(xf * xf, axis=-1, keepdims=True) + EPS)
    return (y * g.astype(jnp.float32)).astype(x.dtype)


def rope(x, positions):
    half = x.shape[-1] // 2
    inv_freq = ROPE_THETA ** (-jnp.arange(half, dtype=jnp.float32) / half)
    ang = positions.astype(jnp.float32)[..., None] * inv_freq
    cos, sin = jnp.cos(ang)[:, :, None, :], jnp.sin(ang)[:, :, None, :]
    xf = x.astype(jnp.float32)
    x1, x2 = xf[..., :half], xf[..., half:]
    return jnp.concatenate([x1 * cos - x2 * sin, x1 * sin + x2 * cos], axis=-1).astype(x.dtype)


def causal_block_attention(score_fn, v):
    B, S, H, dv = v.shape
    n_blocks = S // Q_BLOCK
    kpos = jnp.arange(S)

    def one_block(i):
        start = i * Q_BLOCK
        s = score_fn(start)
        qpos = start + jnp.arange(Q_BLOCK)
        s = jnp.where(kpos[None, :] <= qpos[:, None], s, -jnp.inf)
        p = jax.nn.softmax(s, axis=-1)
        return jnp.einsum('bhqk,bkhd->bqhd', p.astype(v.dtype), v)

    out = lax.map(one_block, jnp.arange(n_blocks))
    return out.transpose(1, 0, 2, 3, 4).reshape(B, S, H, dv)


def hybrid_mixer(h, positions, w_in, q_norm_g, w_uq, kv_norm_g, w_ukv,
                 fox_forget_b, mla_out_g, fox_out_g, w_o):
    B, S, _ = h.shape
    proj = h @ w_in
    cuts = [int(v) for v in np.cumsum(IN_SPLITS)[:-1]]
    c_q, c_kv, k_r, f_q, f_k, f_v, f_logit = jnp.split(proj, cuts, axis=-1)

    q = (rms_norm(c_q, q_norm_g) @ w_uq).reshape(B, S, MLA_HEADS, MLA_NOPE + MLA_ROPE)
    q_nope = q[..., :MLA_NOPE]
    q_rope = rope(q[..., MLA_NOPE:], positions)
    kv = (rms_norm(c_kv, kv_norm_g) @ w_ukv).reshape(B, S, MLA_HEADS, MLA_NOPE + MLA_V)
    k_nope, v_mla = kv[..., :MLA_NOPE], kv[..., MLA_NOPE:]
    k_rope = rope(k_r[:, :, None, :], positions)[:, :, 0, :]
    mla_scale = (MLA_NOPE + MLA_ROPE) ** -0.5

    def mla_scores(start):
        qn = lax.dynamic_slice_in_dim(q_nope, start, Q_BLOCK, axis=1)
        qr = lax.dynamic_slice_in_dim(q_rope, start, Q_BLOCK, axis=1)
        s = (jnp.einsum('bqhd,bkhd->bhqk', qn, k_nope, preferred_element_type=jnp.float32)
             + jnp.einsum('bqhr,bkr->bhqk', qr, k_rope, preferred_element_type=jnp.float32))
        return s * mla_scale

    o_mla = causal_block_attention(mla_scores, v_mla).reshape(B, S, MLA_HEADS * MLA_V)

    fq = f_q.reshape(B, S, FOX_HEADS, FOX_HEAD_DIM)
    fk = f_k.reshape(B, S, FOX_HEADS, FOX_HEAD_DIM)
    fv = f_v.reshape(B, S, FOX_HEADS, FOX_HEAD_DIM)
    log_f = jax.nn.log_sigmoid(f_logit.astype(jnp.float32) + fox_forget_b.astype(jnp.float32))
    F = jnp.cumsum(log_f, axis=1).transpose(0, 2, 1)
    fox_scale = FOX_HEAD_DIM ** -0.5

    def fox_scores(start):
        qb = lax.dynamic_slice_in_dim(fq, start, Q_BLOCK, axis=1)
        Fq = lax.dynamic_slice_in_dim(F, start, Q_BLOCK, axis=2)
        s = jnp.einsum('bqhd,bkhd->bhqk', qb, fk, preferred_element_type=jnp.float32)
        return s * fox_scale + Fq[..., None] - F[:, :, None, :]

    o_fox = causal_block_attention(fox_scores, fv).reshape(B, S, FOX_W)

    o = jnp.concatenate([rms_norm(o_mla, mla_out_g), rms_norm(o_fox, fox_out_g)], axis=-1)
    return o @ w_o


def swiglu(h, w_gate, w_up, w_down):
    return (jax.nn.silu(h @ w_gate) * (h @ w_up)) @ w_down


def moe_swiglu(h, router_w, w_gate, w_up, w_down):
    B, S, D = h.shape
    t = h.reshape(B * S, D)
    logits = (t @ router_w).astype(jnp.float32)
    top_v, top_i = lax.top_k(logits, TOP_K)
    top_w = jax.nn.softmax(top_v, axis=-1)
    gates = jnp.sum(jax.nn.one_hot(top_i, N_EXPERTS, dtype=jnp.float32) * top_w[..., None], axis=1)
    gates = gates.astype(t.dtype)
    y = jnp.zeros_like(t)
    for e in range(N_EXPERTS):
        y = y + gates[:, e:e + 1] * swiglu(t, w_gate[e], w_up[e], w_down[e])
    return y.reshape(B, S, D)


def modulate(h, shift, scale):
    return h * (1 + scale[:, None, :]) + shift[:, None, :]


def setup_inputs(seed: int = 0) -> dict:
    key = jax.random.key(seed)
    ks = jax.random.split(key, 24)
    L, LD, LM = DEPTH, (DEPTH + 1) // 2, DEPTH // 2
    D, F, E = D_MODEL, D_FF, N_EXPERTS

    def nrm(k, shape, scale):
        return jax.random.normal(k, shape, jnp.float32) * scale

    def gain(k, shape):
        return 1.0 + 0.05 * jax.random.normal(k, shape, jnp.float32)

    positions = (jnp.arange(SEQ, dtype=jnp.int32)[None, :]
                 + jax.random.randint(ks[2], (BATCH, 1), 0, 1024, dtype=jnp.int32))
    return {
        "x": nrm(ks[0], (BATCH, SEQ, D), 1.0),
        "c": nrm(ks[1], (BATCH, D), 1.0),
        "positions": positions,
        "ada_w": nrm(ks[3], (L, D, 6 * D), 0.5 * D ** -0.5),
        "ada_b": nrm(ks[4], (L, 6 * D), 0.02),
        "attn_norm_g": gain(ks[5], (L, D)),
        "w_in": nrm(ks[6], (L, D, IN_COLS), D ** -0.5),
        "q_norm_g": gain(ks[7], (L, MLA_Q_LORA)),
        "w_uq": nrm(ks[8], (L, MLA_Q_LORA, MLA_HEADS * (MLA_NOPE + MLA_ROPE)), MLA_Q_LORA ** -0.5),
        "kv_norm_g": gain(ks[9], (L, MLA_KV_LORA)),
        "w_ukv": nrm(ks[10], (L, MLA_KV_LORA, MLA_HEADS * (MLA_NOPE + MLA_V)), MLA_KV_LORA ** -0.5),
        "fox_forget_b": jax.random.uniform(ks[11], (L, FOX_HEADS), jnp.float32, 1.0, 4.0),
        "mla_out_g": gain(ks[12], (L, MLA_HEADS * MLA_V)),
        "fox_out_g": gain(ks[13], (L, FOX_W)),
        "w_o": nrm(ks[14], (L, MIX_WIDTH, D), MIX_WIDTH ** -0.5),
        "ffn_norm_g": gain(ks[15], (L, D)),
        "dense_w_gate": nrm(ks[16], (LD, D, F), D ** -0.5),
        "dense_w_up": nrm(ks[17], (LD, D, F), D ** -0.5),
        "dense_w_down": nrm(ks[18], (LD, F, D), F ** -0.5),
        "router_w": nrm(ks[19], (LM, D, E), D ** -0.5),
        "moe_w_gate": nrm(ks[20], (LM, E, D, F), D ** -0.5),
        "moe_w_up": nrm(ks[21], (LM, E, D, F), D ** -0.5),
        "moe_w_down": nrm(ks[22], (LM, E, F, D), F ** -0.5),
        "final_norm_g": gain(ks[23], (D,)),
    }


def reference(x, c, positions, ada_w, ada_b, attn_norm_g, w_in, q_norm_g, w_uq,
              kv_norm_g, w_ukv, fox_forget_b, mla_out_g, fox_out_g, w_o, ffn_norm_g,
              dense_w_gate, dense_w_up, dense_w_down, router_w, moe_w_gate, moe_w_up,
              moe_w_down, final_norm_g):
    c_act = jax.nn.silu(c)
    for l in range(DEPTH):
        mod = c_act @ ada_w[l] + ada_b[l]
        sh_a, sc_a, g_a, sh_f, sc_f, g_f = jnp.split(mod, 6, axis=-1)
        h = modulate(rms_norm(x, attn_norm_g[l]), sh_a, sc_a)
        mix = hybrid_mixer(h, positions, w_in[l], q_norm_g[l], w_uq[l], kv_norm_g[l],
                           w_ukv[l], fox_forget_b[l], mla_out_g[l], fox_out_g[l], w_o[l])
        x = x + g_a[:, None, :] * mix
        h = modulate(rms_norm(x, ffn_norm_g[l]), sh_f, sc_f)
        j = l // 2
        if l % 2 == 0:
            f = swiglu(h, dense_w_gate[j], dense_w_up[j], dense_w_down[j])
        else:
            f = moe_swiglu(h, router_w[j], moe_w_gate[j], moe_w_up[j], moe_w_down[j])
        x = x + g_f[:, None, :] * f
    return rms_norm(x, final_norm_g)
```

```python
import math
from contextlib import ExitStack

import numpy as np
import concourse.bass as bass
import concourse.mybir as mybir
from concourse.bass_utils import run_bass_kernel_spmd

F32 = mybir.dt.float32
BF16 = mybir.dt.bfloat16
I32 = mybir.dt.int32
AF = mybir.ActivationFunctionType
ALU = mybir.AluOpType
AX = mybir.AxisListType

D = 1024
KC = 8
NH = 8
DFF = 3584
NFG = 7
NE = 8
WIN_COLS = 2056
EPS = 1e-6
NCOL = 170
LCOLS = 75
CONSTF = 128 + 128 + 2048 + 96 + 1024
MLA_SCALE = 96 ** -0.5
TWO_PI = 2.0 * math.pi

DSIZE = {F32: 4, BF16: 2, I32: 4}


class Op:
    __slots__ = ("eng", "fn", "deps", "sem", "ticket", "signal", "is_dma", "idx")


class _Rec:
    def __init__(self):
        self.calls = []

    def __getattr__(self, name):
        def f(*a, **k):
            self.calls.append((name, a, k))
            return None
        return f


class Prog:
    ENGS = ("pe", "act", "dve", "pool", "sp")

    def __init__(self, nc, es):
        self.nc = nc
        self.es = es
        self.ops = {e: [] for e in self.ENGS}
        self.semh = {}
        self.dmacount = {}
        self.lastw = {}
        self.readers = {}
        self.lastdma = {}
        self.pending = {e: [] for e in self.ENGS}
        self.dma_since_bar = []
        self.final_deps = []
        self.n = 0

    def sem(self, name):
        if name not in self.semh:
            self.semh[name] = self.es.enter_context(self.nc.semaphore("s%d" % len(self.semh)))
        return self.semh[name]

    def add(self, eng, fn, reads=(), writes=(), dma_key=None):
        op = Op()
        op.eng = eng
        rec = _Rec()
        fn(rec)
        assert len(rec.calls) == 1
        cname, cargs, ckw = rec.calls[0]
        op.fn = lambda e, _n=cname, _a=cargs, _k=ckw: getattr(e, _n)(*_a, **_k)
        op.is_dma = dma_key is not None
        op.signal = op.is_dma
        op.ticket = None
        op.idx = self.n
        self.n += 1
        deps = {}
        for r in reads:
            w = self.lastw.get(r)
            if w is not None:
                deps[id(w)] = w
        for w_ in writes:
            lw = self.lastw.get(w_)
            if lw is not None:
                deps[id(lw)] = lw
            for rd in self.readers.get(w_, {}).values():
                deps[id(rd)] = rd
        if op.is_dma:
            ld = self.lastdma.get(dma_key)
            if ld is not None:
                deps[id(ld)] = ld
        for p in self.pending[eng]:
            deps[id(p)] = p
        self.pending[eng] = []
        deps.pop(id(op), None)
        if op.is_dma:
            op.sem = "d:" + dma_key
            c = self.dmacount.get(op.sem, 0) + 16
            self.dmacount[op.sem] = c
            op.ticket = c
            self.lastdma[dma_key] = op
            self.dma_since_bar.append(op)
        else:
            op.sem = "e:" + eng
        final = []
        for d in deps.values():
            if (not d.is_dma) and d.eng == "pe" and eng == "pe" and not op.is_dma:
                continue
            d.signal = True
            final.append(d)
        op.deps = final
        rk = op.sem
        for r in reads:
            self.readers.setdefault(r, {})[rk] = op
        for w_ in writes:
            self.lastw[w_] = op
            self.readers[w_] = {}
        self.ops[eng].append(op)
        return op

    def barrier(self):
        deps = []
        for e in self.ENGS:
            if self.ops[e]:
                deps.append(self.ops[e][-1])
        deps.extend(self.dma_since_bar)
        self.dma_since_bar = []
        for d in deps:
            d.signal = True
        for e in self.ENGS:
            self.pending[e] = list(deps)

    def finish(self):
        self.barrier()
        self.final_deps = list(self.pending["sp"])

    def emit(self):
        nc = self.nc
        for e in self.ENGS:
            c = 0
            for op in self.ops[e]:
                if not op.is_dma and op.signal:
                    c += 1
                    op.ticket = c
        handles = {"pe": "tensor", "act": "scalar", "dve": "vector", "pool": "gpsimd", "sp": "sync"}
        import os
        for i_ in range(int(os.environ.get("SEM_SHIFT", "0"))):
            self.sem("dummy%d" % i_)
        names = []
        for e in self.ENGS:
            for op in self.ops[e]:
                if op.signal and op.sem not in names:
                    names.append(op.sem)
        hs = [self.sem(nm) for nm in names]
        with nc.Block() as b0:
            b0.sync(lambda eng: [eng.sem_clear(h) for h in hs])

        def run(ename, eng):
            waited = {}

            def do_waits(deps):
                need = {}
                for d in deps:
                    if d.ticket is None:
                        continue
                    if need.get(d.sem, 0) < d.ticket:
                        need[d.sem] = d.ticket
                for s, v in need.items():
                    if waited.get(s, 0) < v:
                        eng.wait_ge(self.sem(s), v)
                        waited[s] = v

            for op in self.ops[ename]:
                do_waits(op.deps)
                ins = op.fn(eng)
                if op.signal:
                    ins.then_inc(self.sem(op.sem), 16 if op.is_dma else 1)
            if ename == "sp":
                do_waits(self.final_deps)

        with nc.Block() as block:
            for ename in self.ENGS:
                getattr(block, handles[ename])(lambda eng, _n=ename: run(_n, eng))


class Arena:
    def __init__(self, nc, limit):
        self.nc = nc
        self.off = 16512
        self.limit = 16512 + limit
        self.cnt = 0

    def alloc(self, name, shape, dtype):
        n = 1
        for s in shape[1:]:
            n *= s
        nbytes = n * DSIZE[dtype]
        nbytes = (nbytes + 63) // 64 * 64
        assert self.off + nbytes <= self.limit, (name, self.off, nbytes, self.limit)
        self.cnt += 1
        t = self.nc.alloc_sbuf_tensor_at("%s_%d" % (name, self.cnt), list(shape), dtype, offset=self.off)
        self.off += nbytes
        return t

    def mark(self):
        return self.off

    def release(self, m):
        self.off = m


class _Stop(Exception):
    pass


def build_program(T, depth, moe_layers, debug=False, stop=None):
    nc_holder = []
    try:
        return _build_program(T, depth, moe_layers, debug, stop, nc_holder)
    except _Stop:
        nc, es, P = nc_holder
        P.finish()
        P.emit()
        es.close()
        return nc, es


def _build_program(T, depth, moe_layers, debug, stop, nc_holder):
    NCH = T // 512
    NT = T // 128
    nc = bass.Bass("TRN2", target_bir_lowering=False)
    es = ExitStack()
    P = Prog(nc, es)
    nc_holder.extend([nc, es, P])

    def din(name, shape, dt=F32):
        return nc.dram_tensor(name, list(shape), dt, kind="ExternalInput").ap()

    def dscr(name, shape, dt):
        return nc.dram_tensor(name, list(shape), dt, kind=("ExternalOutput" if debug else "Internal")).ap()

    x_d = din("x", [T, D])
    posb_d = din("posb", [128, T], I32)
    cols_d = din("cols", [128, NCOL])
    constf_d = din("constf", [128, CONSTF])
    adaw_d = din("ada_w", [depth, D, 6 * D])
    win_d = din("w_in", [depth, D, WIN_COLS])
    wqa_d = din("w_uqa", [depth, 256, 768])
    wqb_d = din("w_uqb", [depth, 256, 768])
    wkv_d = din("w_ukv", [depth, 128, 1024])
    wo_d = din("w_o", [depth, D, D])
    n_moe = max(1, len(moe_layers))
    n_dense = max(1, depth - len(moe_layers))
    dg_d = din("dense_w_gate", [n_dense, D, DFF])
    du_d = din("dense_w_up", [n_dense, D, DFF])
    dd_d = din("dense_w_down", [n_dense, DFF, D])
    rw_d = din("router_w", [n_moe, D, NE])
    mg_d = din("moe_w_gate", [n_moe, NE, D, DFF])
    mu_d = din("moe_w_up", [n_moe, NE, D, DFF])
    md_d = din("moe_w_down", [n_moe, NE, DFF, D])
    out_d = nc.dram_tensor("out", [T, D], F32, kind="ExternalOutput").ap()

    xT_d = dscr("xT", [KC, 128, T], F32)
    QM_d = dscr("QM", [NH, 96, T], BF16)
    KM_d = dscr("KM", [NH, 96, T], BF16)
    VM_d = dscr("VM", [NH, 128, NT, 65], BF16)
    QF_d = dscr("QF", [4, 128, T], BF16)
    KF_d = dscr("KF", [4, 128, T], BF16)
    QA_d = dscr("QA", [NH, 6, T], BF16)
    KA_d = dscr("KA", [NH, 6, T], BF16)
    VF_d = dscr("VF", [NH, 128, NT, 65], BF16)
    OT_d = dscr("OT", [KC, 128, T], BF16)
    H2_d = dscr("H2", [KC, 128, T], BF16)
    GB_d = dscr("GB", [NE, 128, T], F32)

    A = Arena(nc, 206 * 1024)
    ps = [nc.alloc_psum_tensor("ps%d" % i, [128, 512], F32) for i in range(8)]

    colsb = A.alloc("cols", [128, NCOL], F32)
    identf = A.alloc("identf", [128, 128], F32)
    sell = A.alloc("sell", [128, 96], F32)
    sele = A.alloc("sele", [128, NE, 128], F32)
    identb = A.alloc("identb", [128, 128], BF16)
    onesb = A.alloc("onesb", [128, 128], BF16)
    maskb = A.alloc("maskb", [128, 4, 512], BF16)
    modsb = [A.alloc("mod%d" % l, [128, 48], F32) for l in range(depth)]
    gsa = [A.alloc("gsa%d" % l, [128, 8], F32) for l in range(depth)]
    gsf = [A.alloc("gsf%d" % l, [128, 8], F32) for l in range(depth)]
    cact = A.alloc("cact", [128, 8], F32)
    negfb = A.alloc("negfb", [128, 2], F32)
    sgn2pi = A.alloc("sgn2pi", [128, 1], F32)

    def dma(q, out, in_, key, reads=(), writes=()):
        return P.add(q, lambda e, o=out, i=in_: e.dma_start(out=o, in_=i), reads=reads, writes=writes, dma_key=key)

    def mm(out, lhsT, rhs, start, stop, reads, writes):
        return P.add("pe", lambda e, o=out, l=lhsT, r=rhs, s=start, t=stop: e.matmul(o, l, r, start=s, stop=t),
                     reads=reads, writes=writes)

    def act(out, in_, func, reads, writes, bias=None, scale=None):
        kw = {}
        if bias is not None:
            kw["bias"] = bias
        if scale is not None:
            kw["scale"] = scale
        return P.add("act", lambda e, o=out, i=in_, f=func, k=kw: e.activation(out=o, in_=i, func=f, **k),
                     reads=reads, writes=writes)

    def dve(fn, reads, writes, eng="dve"):
        return P.add(eng, fn, reads=reads, writes=writes)

    def dbg(name, ap, shape, dt, keys):
        if not debug:
            return
        d_ = nc.dram_tensor("dbg_" + name, list(shape), dt, kind="ExternalOutput").ap()
        dma("sp", d_, ap, "dbg_" + name, reads=keys, writes=["dbg_" + name])

    dma("sp", colsb[:], cols_d, "cols", writes=["cols"])
    dma("sp", identf[:], constf_d[:, 0:128], "identf", writes=["identf"])
    dma("sp", sell[:], constf_d[:, 2304:2400], "sell", writes=["sell"])
    dma("sp", sele[:], constf_d[:, 2400:3424].rearrange("p (e m) -> p e m", e=NE), "sele", writes=["sele"])
    dma("pool", identb[:], constf_d[:, 0:128], "identb", writes=["identb"])
    dma("pool", onesb[:], constf_d[:, 128:256], "onesb", writes=["onesb"])
    dma("pool", maskb[:], constf_d[:, 256:2304].rearrange("p (j q) -> p j q", j=4), "maskb", writes=["maskb"])

    act(cact[:], colsb[:, 158:166], AF.Silu, ["cols"], ["cact"])
    dve(lambda e: e.tensor_scalar(out=negfb[:], in0=colsb[:, 168:170], scalar1=-1.0, scalar2=None, op0=ALU.mult),
        ["cols"], ["negfb"])
    dve(lambda e: e.tensor_scalar(out=sgn2pi[:], in0=colsb[:, 167:168], scalar1=TWO_PI * (1.0 - 2e-6), scalar2=None,
                                  op0=ALU.mult), ["cols"], ["sgn2pi"])

    m0 = A.mark()
    awb = [A.alloc("aw%d" % i, [128, KC, 512], F32) for i in range(2)]
    for l in range(depth):
        for jg in range(12):
            b = (l * 12 + jg) % 2
            dma("sp", awb[b][:], adaw_d[l, :, jg * 512:(jg + 1) * 512].rearrange("(k p) n -> p k n", p=128),
                "aw%d" % b, writes=["aw%d" % b])
            for jj in range(4):
                j = jg * 4 + jj
                for k in range(KC):
                    mm(ps[0][:, j:j + 1], awb[b][:, k, jj * 128:(jj + 1) * 128], cact[:, k:k + 1],
                       k == 0, k == KC - 1, ["aw%d" % b, "cact"], ["ps0"])
        c0 = l * LCOLS
        dve(lambda e, l=l, c0=c0: e.tensor_tensor(out=modsb[l][:], in0=ps[0][:, 0:48], in1=colsb[:, c0 + 8:c0 + 56],
                                                  op=ALU.add), ["ps0", "cols"], ["mod%d" % l])
        dve(lambda e, l=l, c0=c0: e.scalar_tensor_tensor(out=gsa[l][:], in0=modsb[l][:, 8:16], scalar=1.0,
                                                         in1=colsb[:, c0:c0 + 8], op0=ALU.add, op1=ALU.mult),
            ["mod%d" % l, "cols"], ["gsa%d" % l])
        dve(lambda e, l=l, c0=c0: e.scalar_tensor_tensor(out=gsf[l][:], in0=modsb[l][:, 32:40], scalar=1.0,
                                                         in1=colsb[:, c0 + 67:c0 + 75], op0=ALU.add, op1=ALU.mult),
            ["mod%d" % l, "cols"], ["gsf%d" % l])
    dbg("mod0", modsb[0][:], [128, 48], F32, ["mod0"])
    dbg("gsa0", gsa[0][:], [128, 8], F32, ["gsa0"])
    dbg("cact", cact[:], [128, 8], F32, ["cact"])
    dbg("cols", colsb[:], [128, NCOL], F32, ["cols"])
    dbg("onesb", onesb[:], [128, 128], BF16, ["onesb"])
    P.barrier()
    A.release(m0)

    def rstd_from(psb, pskey, rs, rskey, n):
        act(rs, psb, AF.Sqrt, [pskey], [rskey], bias=EPS, scale=1.0 / n)
        dve(lambda e, rs=rs: e.reciprocal(out=rs, in_=rs), [rskey], [rskey])

    for l in range(depth):
        c0 = l * LCOLS
        is_moe = l in moe_layers
        m1 = A.mark()
        wi = A.alloc("wi", [128, KC, WIN_COLS], BF16)
        wqa = A.alloc("wqa", [128, 2, 768], BF16)
        wqb = A.alloc("wqb", [128, 2, 768], BF16)
        wkv = A.alloc("wkv", [128, 1024], BF16)
        for k in range(KC):
            dma("pool", wi[:, k, :], win_d[l, k * 128:(k + 1) * 128, :], "wi%d" % k, writes=["wi%d" % k])
        WI = ["wi%d" % k for k in range(KC)]
        dma("pool", wqa[:], wqa_d[l].rearrange("(j p) n -> p j n", p=128), "wqa", writes=["wqa"])
        dma("pool", wqb[:], wqb_d[l].rearrange("(j p) n -> p j n", p=128), "wqb", writes=["wqb"])
        dma("pool", wkv[:], wkv_d[l], "wkv", writes=["wkv"])
        xin = A.alloc("xin", [128, 4, D], F32) if l == 0 else None
        xt = A.alloc("xt", [128, KC, 512], F32)
        sq = A.alloc("sq", [128, KC, 512], BF16)
        tmp = A.alloc("tmp", [128, 2, 512], F32)
        hT = A.alloc("hT", [128, KC, 512], BF16)
        rs = A.alloc("rs", [128, 512], F32)
        cq = A.alloc("cq", [128, 2, 512], F32)
        sqq = A.alloc("sqq", [128, 2, 512], BF16)
        rsq = A.alloc("rsq", [128, 512], F32)
        qn = A.alloc("qn", [128, 2, 512], BF16)
        ckv = A.alloc("ckv", [128, 512], F32)
        sqk = A.alloc("sqk", [128, 512], BF16)
        rsk = A.alloc("rsk", [128, 512], F32)
        kvn = A.alloc("kvn", [128, 512], BF16)
        qt = A.alloc("qt", [96, NH, 512], BF16)
        kt_ = A.alloc("kt", [96, NH, 512], BF16)
        krope = A.alloc("krope", [96, 512], BF16)
        r1 = A.alloc("r1", [96, 512], F32)
        r2 = A.alloc("r2", [96, 512], F32)
        vm = A.alloc("vm", [128, NH, 4, 65], BF16)
        vf = A.alloc("vf", [128, NH, 4, 65], BF16)
        fq = A.alloc("fq", [128, 4, 512], BF16)
        fk = A.alloc("fk", [128, 4, 512], BF16)
        posi = A.alloc("posi", [128, 512], I32)
        rr = A.alloc("rr", [128, 512], F32)
        ri = A.alloc("ri", [128, 512], I32)
        rf = A.alloc("rf", [128, 512], F32)
        ff = A.alloc("ff", [128, 512], F32)
        gg = A.alloc("gg", [128, 512], F32)
        cosb = A.alloc("cosb", [128, 512], F32)
        sinb = A.alloc("sinb", [128, 512], F32)
        ez = A.alloc("ez", [8, 512], F32)
        lp = A.alloc("lp", [8, 512], F32)
        ones8 = A.alloc("ones8", [8, 512], F32)
        Fc = [A.alloc("Fc%d" % i, [8, 512], F32) for i in range(2)]
        fr = A.alloc("fr", [8, 512], F32)
        fb = A.alloc("fb", [8, 512], BF16)
        augq = A.alloc("augq", [8, 6, 512], BF16)
        augk = A.alloc("augk", [8, 6, 512], BF16)

        dve(lambda e: e.memset(vm[:], 1.0), [], ["vm"])
        dve(lambda e: e.memset(vf[:], 1.0), [], ["vf"])
        dve(lambda e: e.memset(ones8[:], 1.0), [], ["ones8"])
        dve(lambda e: e.memset(augq[:], 1.0), [], ["augq"])
        dve(lambda e: e.memset(augk[:], 1.0), [], ["augk"])

        for c in range(NCH):
            cs = slice(c * 512, (c + 1) * 512)
            if l == 0:
                dma("sp", xin[:], x_d[cs, :].rearrange("(t p) d -> p t d", p=128), "xin", writes=["xin"])
                for k in range(KC):
                    pb = k % 2
                    for tt in range(4):
                        mm(ps[pb][:, tt * 128:(tt + 1) * 128], xin[:, tt, k * 128:(k + 1) * 128], identf[:], True, True,
                           ["xin", "identf"], ["ps%d" % pb])
                    act(xt[:, k, :], ps[pb][:], AF.Copy, ["ps%d" % pb], ["xt"])
                dma("sp", xT_d[:, :, cs].rearrange("k p t -> p k t"), xt[:], "xt", reads=["xt"], writes=["xT.%d" % c])
            else:
                dma("sp", xt[:], xT_d[:, :, cs].rearrange("k p t -> p k t"), "xt", reads=["xT.%d" % c], writes=["xt"])
            act(sq[:], xt[:], AF.Square, ["xt"], ["sq"])
            for k in range(KC):
                mm(ps[2][:], onesb[:], sq[:, k, :], k == 0, k == KC - 1, ["onesb", "sq"], ["ps2"])
            rstd_from(ps[2][:], "ps2", rs[:], "rs", D)
            for k in range(KC):
                dve(lambda e, k=k: e.tensor_tensor(out=tmp[:, k % 2, :], in0=xt[:, k, :], in1=rs[:], op=ALU.mult),
                    ["xt", "rs"], ["tmp%d" % (k % 2)])
                act(hT[:, k, :], tmp[:, k % 2, :], AF.Identity, ["tmp%d" % (k % 2), "gsa%d" % l, "mod%d" % l], ["hT%d" % k],
                    bias=modsb[l][:, k:k + 1], scale=gsa[l][:, k:k + 1])
            HT = ["hT%d" % k for k in range(KC)]
            if c == 0 and l == 0:
                dbg("xt", xt[:], [128, KC, 512], F32, ["xt"])
                if stop == "xt":
                    raise _Stop()
                dbg("sq", sq[:], [128, KC, 512], BF16, ["sq"])
                dbg("rs", rs[:], [128, 512], F32, ["rs"])
                dbg("hT", hT[:], [128, KC, 512], BF16, HT)
                dbg("wi", wi[:], [128, KC, WIN_COLS], BF16, WI)
                if stop == "hT":
                    raise _Stop()

            def proj(pb, col0, ncols, extra_reads=()):
                for k in range(KC):
                    mm(ps[pb][0:ncols, :], wi[:, k, col0:col0 + ncols], hT[:, k, :], k == 0, k == KC - 1,
                       [WI[k], HT[k]] + list(extra_reads), ["ps%d" % pb])

            dma("sp", posi[:], posb_d[:, cs], "posi", writes=["posi"])
            dve(lambda e: e.tensor_copy(out=rr[:], in_=posi[:]), ["posi"], ["rr"])
            dve(lambda e: e.tensor_scalar(out=rr[:], in0=rr[:], scalar1=colsb[:, 166:167], scalar2=1.0 / TWO_PI,
                                          op0=ALU.mult, op1=ALU.mult), ["rr", "cols"], ["rr"])
            for which in range(2):
                dst = sinb if which == 0 else cosb
                dkey = "sinb" if which == 0 else "cosb"
                if which == 1:
                    dve(lambda e: e.tensor_scalar(out=rr[:], in0=rr[:], scalar1=0.25, scalar2=None, op0=ALU.add),
                        ["rr"], ["rr"])
                dve(lambda e: e.tensor_copy(out=ri[:], in_=rr[:]), ["rr"], ["ri"])
                dve(lambda e: e.tensor_copy(out=rf[:], in_=ri[:]), ["ri"], ["rf"])
                dve(lambda e: e.tensor_tensor(out=ff[:], in0=rr[:], in1=rf[:], op=ALU.subtract), ["rr", "rf"], ["ff"])
                dve(lambda e: e.tensor_scalar(out=gg[:], in0=ff[:], scalar1=0.5, scalar2=None, op0=ALU.is_gt),
                    ["ff"], ["gg"])
                dve(lambda e: e.tensor_tensor(out=ff[:], in0=ff[:], in1=gg[:], op=ALU.subtract), ["ff", "gg"], ["ff"])
                dve(lambda e: e.tensor_scalar(out=gg[:], in0=ff[:], scalar1=-0.5, scalar2=None, op0=ALU.is_lt),
                    ["ff"], ["gg"])
                dve(lambda e: e.tensor_tensor(out=ff[:], in0=ff[:], in1=gg[:], op=ALU.add), ["ff", "gg"], ["ff"])
                if which == 0:
                    act(dst[:], ff[:], AF.Sin, ["ff", "sgn2pi"], [dkey], scale=sgn2pi[:, 0:1])
                else:
                    act(dst[:], ff[:], AF.Sin, ["ff"], [dkey], scale=TWO_PI * (1.0 - 2e-6))

            def rope_rows(psa, psb, pka, pkb, dst, dkey):
                dve(lambda e: e.tensor_tensor(out=r1[64:96, :], in0=psa[64:96, :], in1=cosb[64:96, :], op=ALU.mult),
                    [pka, "cosb"], ["r1"])
                dve(lambda e: e.tensor_tensor(out=r2[64:96, :], in0=psb[64:96, :], in1=sinb[64:96, :], op=ALU.mult),
                    [pkb, "sinb"], ["r2"])
                dve(lambda e, dst=dst: e.tensor_tensor(out=dst, in0=r1[64:96, :], in1=r2[64:96, :], op=ALU.add),
                    ["r1", "r2"], [dkey])

            if stop == "rope":
                raise _Stop()
            for j in range(2):
                proj(3 + j, j * 128, 128)
                act(cq[:, j, :], ps[3 + j][:], AF.Copy, ["ps%d" % (3 + j)], ["cq%d" % j])
            if stop == "cq":
                raise _Stop()
            proj(5, 256, 128)
            act(ckv[:], ps[5][:], AF.Copy, ["ps5"], ["ckv"])
            if stop == "ckv":
                raise _Stop()
            proj(6, 320, 96)
            proj(7, 416, 96)
            rope_rows(ps[6], ps[7], "ps6", "ps7", krope[64:96, :], "krope")
            if stop == "krope":
                raise _Stop()
            proj(3, 512, 128)
            act(ez[:], ps[3][0:8, :], AF.Exp, ["ps3", "negfb"], ["ez"], bias=negfb[0:8, l:l + 1], scale=-1.0)
            act(lp[:], ez[:], AF.Ln, ["ez"], ["lp"], bias=1.0)
            cur, prev = Fc[c % 2], Fc[(c + 1) % 2]
            init = 0.0 if c == 0 else prev[:, 511:512]
            dve(lambda e, cur=cur, init=init: e.tensor_tensor_scan(out=cur[:], data0=ones8[:], data1=lp[:], initial=init,
                                                                   op0=ALU.mult, op1=ALU.subtract),
                ["ones8", "lp", "Fc%d" % ((c + 1) % 2)], ["Fc%d" % (c % 2)])
            fkey = "Fc%d" % (c % 2)
            dve(lambda e, cur=cur: e.tensor_copy(out=augq[:, 0, :], in_=cur[:]), [fkey], ["augq"])
            dve(lambda e, cur=cur: e.tensor_tensor(out=fr[:], in0=cur[:], in1=augq[:, 0, :], op=ALU.subtract),
                [fkey, "augq"], ["fr"])
            dve(lambda e: e.tensor_copy(out=augq[:, 1, :], in_=fr[:]), ["fr"], ["augq"])
            dve(lambda e: e.tensor_tensor(out=fr[:], in0=fr[:], in1=augq[:, 1, :], op=ALU.subtract), ["fr", "augq"], ["fr"])
            dve(lambda e: e.tensor_copy(out=augq[:, 2, :], in_=fr[:]), ["fr"], ["augq"])
            for i3 in range(3):
                dve(lambda e, i3=i3: e.tensor_scalar(out=augk[:, 3 + i3, :], in0=augq[:, i3, :], scalar1=-1.0,
                                                     scalar2=None, op0=ALU.mult), ["augq"], ["augk"])
            dma("sp", QA_d[:, :, cs], augq[:], "augq", reads=["augq"], writes=["QA.%d" % c])
            dma("sp", KA_d[:, :, cs], augk[:], "augk", reads=["augk"], writes=["KA.%d" % c])
            if stop == "gates":
                raise _Stop()
            for j in range(4):
                pb = 4 + (j % 2)
                proj(pb, 520 + j * 128, 128)
                act(fq[:, j, :], ps[pb][:], AF.Copy, ["ps%d" % pb], ["fq"], scale=0.125)
            dma("sp", QF_d[:, :, cs].rearrange("j p t -> p j t"), fq[:], "fq", reads=["fq"], writes=["QF.%d" % c])
            for j in range(4):
                pb = 6 + (j % 2)
                proj(pb, 1032 + j * 128, 128)
                dve(lambda e, j=j, pb=pb: e.tensor_copy(out=fk[:, j, :], in_=ps[pb][:]), ["ps%d" % pb], ["fk"])
            dma("sp", KF_d[:, :, cs].rearrange("j p t -> p j t"), fk[:], "fk", reads=["fk"], writes=["KF.%d" % c])
            if stop == "fqk":
                raise _Stop()
            for tt in range(4):
                pb = 3 + (tt % 2)
                for k in range(KC):
                    mm(ps[pb][:], hT[:, k, tt * 128:(tt + 1) * 128], wi[:, k, 1544:2056], k == 0, k == KC - 1,
                       [HT[k], WI[k]], ["ps%d" % pb])
                act(vf[:, :, tt, 0:64], ps[pb][:].rearrange("p (h d) -> p h d", h=NH), AF.Copy, ["ps%d" % pb], ["vf"])
            dma("sp", VF_d[:, :, c * 4:(c + 1) * 4, :].rearrange("h p t d -> p h t d"), vf[:], "vf", reads=["vf"],
                writes=["VF.%d" % c])
            if stop == "fv":
                raise _Stop()
            act(sqq[:], cq[:], AF.Square, ["cq0", "cq1"], ["sqq"])
            for j in range(2):
                mm(ps[2][:], onesb[:], sqq[:, j, :], j == 0, j == 1, ["onesb", "sqq"], ["ps2"])
            rstd_from(ps[2][:], "ps2", rsq[:], "rsq", 256)
            for j in range(2):
                dve(lambda e, j=j: e.scalar_tensor_tensor(out=qn[:, j, :], in0=cq[:, j, :],
                                                          scalar=colsb[:, c0 + 56 + j:c0 + 57 + j], in1=rsq[:],
                                                          op0=ALU.mult, op1=ALU.mult),
                    ["cq%d" % j, "rsq", "cols"], ["qn"])
            for h in range(NH):
                pa, pbb = (4, 5) if h % 2 == 0 else (6, 7)
                for j in range(2):
                    mm(ps[pa][0:96, :], wqa[:, j, h * 96:(h + 1) * 96], qn[:, j, :], j == 0, j == 1, ["wqa", "qn"],
                       ["ps%d" % pa])
                for j in range(2):
                    mm(ps[pbb][0:96, :], wqb[:, j, h * 96:(h + 1) * 96], qn[:, j, :], j == 0, j == 1, ["wqb", "qn"],
                       ["ps%d" % pbb])
                act(qt[0:64, h, :], ps[pa][0:64, :], AF.Copy, ["ps%d" % pa], ["qt.n"])
                rope_rows(ps[pa], ps[pbb], "ps%d" % pa, "ps%d" % pbb, qt[64:96, h, :], "qt.r")
            dma("sp", QM_d[:, :, cs].rearrange("h r t -> r h t"), qt[:], "qt", reads=["qt.n", "qt.r"], writes=["QM.%d" % c])
            if stop == "qpath":
                raise _Stop()
            act(sqk[:], ckv[:], AF.Square, ["ckv"], ["sqk"])
            mm(ps[2][:], onesb[:], sqk[:], True, True, ["onesb", "sqk"], ["ps2"])
            rstd_from(ps[2][:], "ps2", rsk[:], "rsk", 128)
            dve(lambda e: e.scalar_tensor_tensor(out=kvn[:], in0=ckv[:], scalar=colsb[:, c0 + 58:c0 + 59], in1=rsk[:],
                                                 op0=ALU.mult, op1=ALU.mult), ["ckv", "rsk", "cols"], ["kvn"])
            for h in range(NH):
                pb = 3 + (h % 2)
                mm(ps[pb][0:96, :], wkv[:, h * 64:h * 64 + 96], kvn[:], True, True, ["wkv", "kvn"], ["ps%d" % pb])
                act(kt_[0:64, h, :], ps[pb][0:64, :], AF.Copy, ["ps%d" % pb], ["kt.n"])
                P.add("pool", lambda e, h=h: e.tensor_copy(out=kt_[64:96, h, :], in_=krope[64:96, :]),
                      reads=["krope"], writes=["kt.r"])
            dma("sp", KM_d[:, :, cs].rearrange("h r t -> r h t"), kt_[:], "kt", reads=["kt.n", "kt.r"], writes=["KM.%d" % c])
            for tt in range(4):
                pb = 4 + (tt % 2)
                mm(ps[pb][:], kvn[:, tt * 128:(tt + 1) * 128], wkv[:, 512:1024], True, True, ["kvn", "wkv"],
                   ["ps%d" % pb])
                dve(lambda e, tt=tt, pb=pb: e.tensor_copy(out=vm[:, :, tt, 0:64],
                                                          in_=ps[pb][:].rearrange("p (h d) -> p h d", h=NH)),
                    ["ps%d" % pb], ["vm"])
            dma("sp", VM_d[:, :, c * 4:(c + 1) * 4, :].rearrange("h p t d -> p h t d"), vm[:], "vm", reads=["vm"],
                writes=["VM.%d" % c])
        if stop == "p1":
            raise _Stop()
        P.barrier()
        A.release(m1)

        m2 = A.mark()
        Kb = [A.alloc("Kb%d" % i, [96, T], BF16) for i in range(2)]
        Qb = [A.alloc("Qb%d" % i, [96, T], BF16) for i in range(2)]
        Vb = [A.alloc("Vb%d" % i, [128, NT, 65], BF16) for i in range(2)]
        Ob = [A.alloc("Ob%d" % i, [64, T], BF16) for i in range(2)]
        NPT = 5
        Pt = [A.alloc("Pt%d" % i, [128, 512], BF16) for i in range(NPT)]
        Osb = [A.alloc("Osb%d" % i, [65, 512], F32) for i in range(2)]
        ALLC = lambda pre: [pre + ".%d" % c for c in range(NCH)]
        LA = 3
        items = []
        hh = 0
        gcount = 0
        for typ in range(2):
            for h in range(NH):
                for qc in range(NCH):
                    nk = 4 * (qc + 1)
                    for kt in range(nk):
                        items.append(dict(typ=typ, h=h, hh=hh, qc=qc, kt=kt, nk=nk, g=gcount))
                    gcount += 1
                hh += 1
        loaded = set()

        def load_head(it):
            if it["hh"] in loaded:
                return
            loaded.add(it["hh"])
            typ, h, b = it["typ"], it["h"], it["hh"] % 2
            kk, qk, vk = "Kb%d" % b, "Qb%d" % b, "Vb%d" % b
            if typ == 0:
                dma("sp", Kb[b][0:96, :], KM_d[h], kk, reads=ALLC("KM"), writes=[kk + "m", kk + "a"])
                dma("sp", Qb[b][0:96, :], QM_d[h], qk, reads=ALLC("QM"), writes=[qk + "m", qk + "a"])
                dma("sp", Vb[b][:], VM_d[h], vk, reads=ALLC("VM"), writes=[vk])
            else:
                hp, ho = h // 2, (h % 2) * 64
                dma("sp", Kb[b][0:64, :], KF_d[hp, ho:ho + 64, :], kk, reads=ALLC("KF"), writes=[kk + "m"])
                dma("sp", Kb[b][64:70, :], KA_d[h], kk + "a", reads=ALLC("KA"), writes=[kk + "a"])
                dma("sp", Qb[b][0:64, :], QF_d[hp, ho:ho + 64, :], qk, reads=ALLC("QF"), writes=[qk + "m"])
                dma("sp", Qb[b][64:70, :], QA_d[h], qk + "a", reads=ALLC("QA"), writes=[qk + "a"])
                dma("sp", Vb[b][:], VF_d[h], vk, reads=ALLC("VF"), writes=[vk])

        def emit_qk(n, it):
            typ, b, qc, kt = it["typ"], it["hh"] % 2, it["qc"], it["kt"]
            R = 96 if typ == 0 else 70
            j = kt - 4 * qc
            sb, pb = n % 4, n % NPT
            skey = "ps%d" % sb
            kr = ["Kb%dm" % b, "Kb%da" % b]
            qr = ["Qb%dm" % b, "Qb%da" % b]
            c0_ = max(j, 0) * 128
            mm(ps[sb][:, c0_:512], Kb[b][0:R, kt * 128:(kt + 1) * 128], Qb[b][0:R, qc * 512 + c0_:(qc + 1) * 512],
               True, j < 0, kr + qr, [skey])
            if j >= 0:
                mm(ps[sb][:, c0_:c0_ + 128], identb[:], maskb[:, 0, 0:128], False, True, ["identb", "maskb"], [skey])
            act(Pt[pb][:, c0_:512], ps[sb][:, c0_:512], AF.Exp, [skey], ["Pt%d" % pb],
                scale=(MLA_SCALE if typ == 0 else 1.0))

        def emit_pv(n, it):
            b, kt, nk, ob = it["hh"] % 2, it["kt"], it["nk"], it["g"] % 2
            pb = n % NPT
            c0_ = max(kt - 4 * it["qc"], 0) * 128
            mm(ps[4 + ob][0:65, c0_:512], Vb[b][:, kt, :], Pt[pb][:, c0_:512], kt == 0, kt == nk - 1,
               ["Vb%d" % b, "Pt%d" % pb], ["ps%d" % (4 + ob)])

        def epi1(it):
            ob = it["g"] % 2
            dve(lambda e: e.tensor_copy(out=Osb[ob][:], in_=ps[4 + ob][0:65, :]), ["ps%d" % (4 + ob)], ["Osb%d" % ob])
            dve(lambda e: e.reciprocal(out=Osb[ob][64:65, :], in_=Osb[ob][64:65, :]), ["Osb%d" % ob], ["Osb%d" % ob])

        def epi2(it):
            ob, b, qc, typ, h = it["g"] % 2, it["hh"] % 2, it["qc"], it["typ"], it["h"]
            ok = "Ob%d" % b
            mm(ps[6 + ob][0:96, :], sell[0:65, :], Osb[ob][:], True, True, ["sell", "Osb%d" % ob], ["ps%d" % (6 + ob)])
            dve(lambda e: e.tensor_tensor(out=Ob[b][:, qc * 512:(qc + 1) * 512], in0=Osb[ob][0:64, :],
                                          in1=ps[6 + ob][0:64, :], op=ALU.mult),
                ["Osb%d" % ob, "ps%d" % (6 + ob)], [ok])
            if qc == NCH - 1:
                kch = typ * 4 + h // 2
                ho = (h % 2) * 64
                dma("sp", OT_d[kch, ho:ho + 64, :], Ob[b][:], ok, reads=[ok], writes=["OT.%d.%d" % (kch, h % 2)])

        NI = len(items)
        deferred = []
        for n in range(NI + LA):
            if n < NI:
                load_head(items[n])
                emit_qk(n, items[n])
            m_ = n - LA
            if m_ >= 0:
                it = items[m_]
                if it["kt"] == 0 and it["qc"] == 0:
                    nxt = [x for x in items[m_:m_ + 40 * NCH * NCH + 8] if x["hh"] == it["hh"] + 1]
                    if nxt:
                        load_head(nxt[0])
                emit_pv(m_, it)
                if it["kt"] == it["nk"] - 1:
                    epi1(it)
                    deferred.append((n + 2, it))
            while deferred and (deferred[0][0] <= n or n == NI + LA - 1):
                epi2(deferred.pop(0)[1])
        if stop == "p2":
            raise _Stop()
        P.barrier()
        A.release(m2)

        m3 = A.mark()
        wo = A.alloc("wo", [128, KC, D], BF16)
        dma("pool", wo[:], wo_d[l].rearrange("(k p) n -> p k n", p=128), "wo", writes=["wo"])
        ot = [A.alloc("ot%d" % i_, [128, KC, 512], BF16) for i_ in range(2)]
        sqo = [A.alloc("sqo%d" % i_, [128, KC, 512], BF16) for i_ in range(2)]
        on = [A.alloc("on%d" % i_, [128, KC, 512], BF16) for i_ in range(2)]
        xt = [A.alloc("xt3%d" % i_, [128, KC, 512], F32) for i_ in range(2)]
        tmp = A.alloc("tmp3", [128, 2, 512], F32)
        h2 = [A.alloc("h2%d" % i_, [128, KC, 512], BF16) for i_ in range(2)]
        rsm = A.alloc("rsm", [128, 512], F32)
        rsf = A.alloc("rsf", [128, 512], F32)
        rs2 = A.alloc("rs2", [128, 512], F32)
        if is_moe:
            mi = moe_layers.index(l)
            h2f = [A.alloc("h2f%d" % i_, [128, KC, 512], F32) for i_ in range(2)]
            rw = A.alloc("rw", [128, KC, NE], F32)
            dma("sp", rw[:], rw_d[mi].rearrange("(k p) e -> p k e", p=128), "rw", writes=["rw"])
            lg = A.alloc("lg", [128, 4, NE], F32)
            m1t = A.alloc("m1t", [128, 4], F32)
            nm1 = A.alloc("nm1", [128, 4], F32)
            m2t = A.alloc("m2t", [128, 4], F32)
            eq = A.alloc("eq", [128, 4, NE], F32)
            mk = A.alloc("mk", [128, 4, NE], F32)
            selt = A.alloc("selt", [128, 4, NE], F32)
            ex = A.alloc("ex", [128, 4, NE], F32)
            den = A.alloc("den", [128, 4], F32)
            gt = A.alloc("gt", [128, 4, 128], F32)
            gT = A.alloc("gT", [128, 512], F32)
            gb = A.alloc("gb", [128, NE, 512], F32)
            dve(lambda e: e.memset(gt[:], 0.0), [], ["gt"])
        ALLO = ["OT.%d.%d" % (k, s) for k in range(KC) for s in range(2)]
        for c in range(NCH):
            p_ = c % 2
            cs = slice(c * 512, (c + 1) * 512)
            dma("sp", ot[p_][:], OT_d[:, :, cs].rearrange("k p t -> p k t"), "ot%d" % p_, reads=ALLO, writes=["ot%d" % p_])
            dma("sp", xt[p_][:], xT_d[:, :, cs].rearrange("k p t -> p k t"), "xt3%d" % p_, reads=["xT.%d" % c], writes=["xt3%d" % p_])
            if stop == "p3a":
                raise _Stop()
            act(sqo[p_][:], ot[p_][:], AF.Square, ["ot%d" % p_], ["sqo%d" % p_])
            for k in range(4):
                mm(ps[0][:], onesb[:], sqo[p_][:, k, :], k == 0, k == 3, ["onesb", "sqo%d" % p_], ["ps0"])
            for k in range(4):
                mm(ps[1][:], onesb[:], sqo[p_][:, 4 + k, :], k == 0, k == 3, ["onesb", "sqo%d" % p_], ["ps1"])
            if stop == "p3b":
                raise _Stop()
            rstd_from(ps[0][:], "ps0", rsm[:], "rsm", 512)
            rstd_from(ps[1][:], "ps1", rsf[:], "rsf", 512)
            for k in range(KC):
                rsx, rkey = (rsm, "rsm") if k < 4 else (rsf, "rsf")
                gcol = c0 + 59 + k
                dve(lambda e, k=k, rsx=rsx, gcol=gcol: e.scalar_tensor_tensor(
                    out=on[p_][:, k, :], in0=ot[p_][:, k, :], scalar=colsb[:, gcol:gcol + 1], in1=rsx[:], op0=ALU.mult,
                    op1=ALU.mult), ["ot%d" % p_, rkey, "cols"], ["on%d_%d" % (p_, k)])
            if stop == "p3c":
                raise _Stop()
            for i in range(KC):
                pb = 2 + (i % 2)
                for k in range(KC):
                    mm(ps[pb][:], wo[:, k, i * 128:(i + 1) * 128], on[p_][:, k, :], k == 0, k == KC - 1,
                       ["wo", "on%d_%d" % (p_, k)], ["ps%d" % pb])
                dve(lambda e, i=i, pb=pb: e.scalar_tensor_tensor(out=xt[p_][:, i, :], in0=ps[pb][:],
                                                                 scalar=modsb[l][:, 16 + i:17 + i], in1=xt[p_][:, i, :],
                                                                 op0=ALU.mult, op1=ALU.add),
                    ["ps%d" % pb, "mod%d" % l, "xt3%d" % p_], ["xt3%d" % p_])
            dma("sp", xT_d[:, :, cs].rearrange("k p t -> p k t"), xt[p_][:], "xt3%d" % p_, reads=["xt3%d" % p_], writes=["xT.%d" % c])
            if stop == "p3e":
                raise _Stop()
            act(sqo[p_][:], xt[p_][:], AF.Square, ["xt3%d" % p_], ["sqo%d" % p_])
            for k in range(KC):
                mm(ps[4][:], onesb[:], sqo[p_][:, k, :], k == 0, k == KC - 1, ["onesb", "sqo%d" % p_], ["ps4"])
            rstd_from(ps[4][:], "ps4", rs2[:], "rs2", D)
            for k in range(KC):
                dve(lambda e, k=k: e.tensor_tensor(out=tmp[:, k % 2, :], in0=xt[p_][:, k, :], in1=rs2[:], op=ALU.mult),
                    ["xt3%d" % p_, "rs2"], ["tmp3%d" % (k % 2)])
                if is_moe:
                    act(h2f[p_][:, k, :], tmp[:, k % 2, :], AF.Identity, ["tmp3%d" % (k % 2), "gsf%d" % l, "mod%d" % l],
                        ["h2f%d_%d" % (p_, k)], bias=modsb[l][:, 24 + k:25 + k], scale=gsf[l][:, k:k + 1])
                    P.add("pool", lambda e, k=k: e.tensor_copy(out=h2[p_][:, k, :], in_=h2f[p_][:, k, :]),
                          reads=["h2f%d_%d" % (p_, k)], writes=["h2_%d" % p_])
                else:
                    act(h2[p_][:, k, :], tmp[:, k % 2, :], AF.Identity, ["tmp3%d" % (k % 2), "gsf%d" % l, "mod%d" % l], ["h2_%d" % p_],
                        bias=modsb[l][:, 24 + k:25 + k], scale=gsf[l][:, k:k + 1])
            dma("sp", H2_d[:, :, cs].rearrange("k p t -> p k t"), h2[p_][:], "h2_%d" % p_, reads=["h2_%d" % p_], writes=["H2.%d" % c])
            if is_moe:
                for tt in range(4):
                    for k in range(KC):
                        mm(ps[5][:, tt * NE:(tt + 1) * NE], h2f[p_][:, k, tt * 128:(tt + 1) * 128], rw[:, k, :], k == 0,
                           k == KC - 1, ["h2f%d_%d" % (p_, k), "rw"], ["ps5"])
                dve(lambda e: e.tensor_copy(out=lg[:], in_=ps[5][:, 0:4 * NE].rearrange("p (t e) -> p t e", t=4)),
                    ["ps5"], ["lg"])
                dve(lambda e: e.tensor_reduce(out=m1t[:], in_=lg[:], axis=AX.X, op=ALU.max), ["lg"], ["m1t"])
                dve(lambda e: e.tensor_scalar(out=nm1[:], in0=m1t[:], scalar1=-1.0, scalar2=None, op0=ALU.mult),
                    ["m1t"], ["nm1"])
                for tt in range(4):
                    dve(lambda e, tt=tt: e.tensor_scalar(out=eq[:, tt, :], in0=lg[:, tt, :], scalar1=m1t[:, tt:tt + 1],
                                                         scalar2=None, op0=ALU.is_equal), ["lg", "m1t"], ["eq"])
                dve(lambda e: e.scalar_tensor_tensor(out=mk[:], in0=eq[:], scalar=-1e30, in1=lg[:], op0=ALU.mult,
                                                     op1=ALU.add), ["eq", "lg"], ["mk"])
                dve(lambda e: e.tensor_reduce(out=m2t[:], in_=mk[:], axis=AX.X, op=ALU.max), ["mk"], ["m2t"])
                for tt in range(4):
                    dve(lambda e, tt=tt: e.tensor_scalar(out=selt[:, tt, :], in0=lg[:, tt, :], scalar1=m2t[:, tt:tt + 1],
                                                         scalar2=None, op0=ALU.is_ge), ["lg", "m2t"], ["selt"])
                    act(ex[:, tt, :], lg[:, tt, :], AF.Exp, ["lg", "nm1"], ["ex"], bias=nm1[:, tt:tt + 1], scale=1.0)
                    act(den[:, tt:tt + 1], m2t[:, tt:tt + 1], AF.Exp, ["m2t", "nm1"], ["den"], bias=nm1[:, tt:tt + 1],
                        scale=1.0)
                dve(lambda e: e.tensor_scalar(out=den[:], in0=den[:], scalar1=1.0, scalar2=None, op0=ALU.add),
                    ["den"], ["den"])
                dve(lambda e: e.reciprocal(out=den[:], in_=den[:]), ["den"], ["den"])
                for tt in range(4):
                    dve(lambda e, tt=tt: e.scalar_tensor_tensor(out=gt[:, tt, 0:NE], in0=ex[:, tt, :],
                                                                scalar=den[:, tt:tt + 1], in1=selt[:, tt, :],
                                                                op0=ALU.mult, op1=ALU.mult),
                        ["ex", "den", "selt"], ["gt"])
                for tt in range(4):
                    mm(ps[6][:, tt * 128:(tt + 1) * 128], gt[:, tt, :], identf[:], True, True, ["gt", "identf"], ["ps6"])
                dve(lambda e: e.tensor_copy(out=gT[:], in_=ps[6][:]), ["ps6"], ["gT"])
                for ex_ in range(NE):
                    pb = 6 + (ex_ % 2)
                    mm(ps[pb][:], sele[:, ex_, :], gT[:], True, True, ["sele", "gT"], ["ps%d" % pb])
                    act(gb[:, ex_, :], ps[pb][:], AF.Copy, ["ps%d" % pb], ["gb"])
                dma("sp", GB_d[:, :, cs].rearrange("e p t -> p e t"), gb[:], "gb", reads=["gb"], writes=["GB.%d" % c])
        if stop == "p3":
            raise _Stop()
        P.barrier()
        A.release(m3)

        m4 = A.mark()
        TB = min(T, 2048)
        NSB = TB // 512
        h2b = A.alloc("h2b", [128, KC, TB], BF16)
        yacc = A.alloc("yacc", [128, KC, TB], F32)
        wg = [A.alloc("wg%d" % i, [128, KC, 512], BF16) for i in range(2)]
        wu = [A.alloc("wu%d" % i, [128, KC, 512], BF16) for i in range(2)]
        wd = [A.alloc("wd%d" % i, [128, 4, D], BF16) for i in range(2)]
        actb = [A.alloc("actb%d" % i, [128, 4, 512], BF16) for i in range(2)]
        sg = [A.alloc("sg%d" % i, [128, 512], F32) for i in range(2)]
        ta = [A.alloc("ta%d" % i, [128, 512], F32) for i in range(2)] if is_moe else None
        gbe = [A.alloc("gbe%d" % i, [128, TB], F32) for i in range(2)] if is_moe else None
        xt = A.alloc("xt4", [128, KC, 512], F32)
        nexp = NE if is_moe else 1
        wcount = 0
        gcount = 0
        for blk in range(T // TB):
            bs = slice(blk * TB, (blk + 1) * TB)
            chunks = range(blk * NSB, (blk + 1) * NSB)
            dma("sp", h2b[:], H2_d[:, :, bs].rearrange("k p t -> p k t"), "h2b",
                reads=["H2.%d" % c for c in chunks], writes=["h2b"])
            for ei in range(nexp):
                if is_moe:
                    mi = moe_layers.index(l)
                    Wg, Wu, Wd = mg_d[mi, ei], mu_d[mi, ei], md_d[mi, ei]
                    gbb = gcount % 2
                    gcount += 1
                    dma("sp", gbe[gbb][:], GB_d[ei, :, bs], "gbe%d" % gbb, reads=["GB.%d" % c for c in chunks],
                        writes=["gbe%d" % gbb])
                else:
                    di = [x for x in range(depth) if x not in moe_layers].index(l)
                    Wg, Wu, Wd = dg_d[di], du_d[di], dd_d[di]
                for fg in range(NFG):
                    wb = wcount % 2
                    wcount += 1
                    fs = slice(fg * 512, (fg + 1) * 512)
                    dma("pool", wg[wb][:], Wg[:, fs].rearrange("(k p) n -> p k n", p=128), "wg%d" % wb,
                        writes=["wg%d" % wb])
                    dma("pool", wu[wb][:], Wu[:, fs].rearrange("(k p) n -> p k n", p=128), "wu%d" % wb,
                        writes=["wu%d" % wb])
                    dma("pool", wd[wb][:], Wd[fs, :].rearrange("(j p) n -> p j n", p=128), "wd%d" % wb,
                        writes=["wd%d" % wb])
                    for sbi in range(NSB):
                        ss = slice(sbi * 512, (sbi + 1) * 512)
                        ab = (wcount * NSB + sbi) % 2
                        akey = "actb%d" % ab
                        for jj in range(4):
                            g_i = jj % 2
                            pg, pu = g_i, 2 + g_i
                            for k in range(KC):
                                mm(ps[pg][:], wg[wb][:, k, jj * 128:(jj + 1) * 128], h2b[:, k, ss], k == 0, k == KC - 1,
                                   ["wg%d" % wb, "h2b"], ["ps%d" % pg])
                            for k in range(KC):
                                mm(ps[pu][:], wu[wb][:, k, jj * 128:(jj + 1) * 128], h2b[:, k, ss], k == 0, k == KC - 1,
                                   ["wu%d" % wb, "h2b"], ["ps%d" % pu])
                            act(sg[g_i][:], ps[pg][:], AF.Silu, ["ps%d" % pg], ["sg%d" % g_i])
                            if is_moe:
                                dve(lambda e, g_i=g_i, pu=pu: e.tensor_tensor(out=ta[g_i][:], in0=sg[g_i][:],
                                                                              in1=ps[pu][:], op=ALU.mult),
                                    ["sg%d" % g_i, "ps%d" % pu], ["ta%d" % g_i])
                                dve(lambda e, g_i=g_i, ab=ab, jj=jj, gbb=gbb, ss=ss: e.tensor_tensor(
                                    out=actb[ab][:, jj, :], in0=ta[g_i][:], in1=gbe[gbb][:, ss], op=ALU.mult),
                                    ["ta%d" % g_i, "gbe%d" % gbb], [akey + ".%d" % jj])
                            else:
                                dve(lambda e, g_i=g_i, pu=pu, ab=ab, jj=jj: e.tensor_tensor(
                                    out=actb[ab][:, jj, :], in0=sg[g_i][:], in1=ps[pu][:], op=ALU.mult),
                                    ["sg%d" % g_i, "ps%d" % pu], [akey + ".%d" % jj])
                        first = (ei == 0 and fg == 0)
                        for i in range(KC):
                            py = 4 + (i % 2)
                            for jj in range(4):
                                mm(ps[py][:], wd[wb][:, jj, i * 128:(i + 1) * 128], actb[ab][:, jj, :], jj == 0, jj == 3,
                                   ["wd%d" % wb, akey + ".%d" % jj], ["ps%d" % py])
                            ykey = "yacc.%d.%d" % (i, sbi)
                            if first:
                                act(yacc[:, i, ss], ps[py][:], AF.Copy, ["ps%d" % py], [ykey])
                            else:
                                dve(lambda e, i=i, py=py, ss=ss: e.tensor_tensor(out=yacc[:, i, ss], in0=ps[py][:],
                                                                                 in1=yacc[:, i, ss], op=ALU.add),
                                    ["ps%d" % py, ykey], [ykey])
            for sbi in range(NSB):
                c = blk * NSB + sbi
                cs = slice(c * 512, (c + 1) * 512)
                ss = slice(sbi * 512, (sbi + 1) * 512)
                dma("sp", xt[:], xT_d[:, :, cs].rearrange("k p t -> p k t"), "xt4", reads=["xT.%d" % c], writes=["xt4"])
                for i in range(KC):
                    dve(lambda e, i=i, ss=ss: e.scalar_tensor_tensor(out=xt[:, i, :], in0=yacc[:, i, ss],
                                                                     scalar=modsb[l][:, 40 + i:41 + i], in1=xt[:, i, :],
                                                                     op0=ALU.mult, op1=ALU.add),
                        ["yacc.%d.%d" % (i, sbi), "mod%d" % l, "xt4"], ["xt4"])
                dma("sp", xT_d[:, :, cs].rearrange("k p t -> p k t"), xt[:], "xt4", reads=["xt4"], writes=["xT.%d" % c])
        if stop == "p4":
            raise _Stop()
        P.barrier()
        A.release(m4)

    xt = A.alloc("xt5", [128, KC, 512], F32)
    sq = A.alloc("sq5", [128, KC, 512], BF16)
    rs = A.alloc("rs5", [128, 512], F32)
    yT = A.alloc("yT", [128, KC, 512], F32)
    yo = [A.alloc("yo%d" % i, [128, D], F32) for i in range(2)]
    ocount = 0
    for c in range(NCH):
        cs = slice(c * 512, (c + 1) * 512)
        dma("sp", xt[:], xT_d[:, :, cs].rearrange("k p t -> p k t"), "xt5", reads=["xT.%d" % c], writes=["xt5"])
        act(sq[:], xt[:], AF.Square, ["xt5"], ["sq5"])
        for k in range(KC):
            mm(ps[0][:], onesb[:], sq[:, k, :], k == 0, k == KC - 1, ["onesb", "sq5"], ["ps0"])
        rstd_from(ps[0][:], "ps0", rs[:], "rs5", D)
        for k in range(KC):
            dve(lambda e, k=k: e.scalar_tensor_tensor(out=yT[:, k, :], in0=xt[:, k, :], scalar=colsb[:, 150 + k:151 + k],
                                                      in1=rs[:], op0=ALU.mult, op1=ALU.mult),
                ["xt5", "rs5", "cols"], ["yT"])
        for tt in range(4):
            ob = ocount % 2
            ocount += 1
            for half in range(2):
                pb = 2 + 2 * (tt % 2) + half
                for kq in range(4):
                    k = half * 4 + kq
                    mm(ps[pb][:, kq * 128:(kq + 1) * 128], yT[:, k, tt * 128:(tt + 1) * 128], identf[:], True, True,
                       ["yT", "identf"], ["ps%d" % pb])
                if half == 0:
                    act(yo[ob][:, 0:512], ps[pb][:], AF.Copy, ["ps%d" % pb], ["yo%d" % ob])
                else:
                    dve(lambda e, ob=ob, pb=pb: e.tensor_copy(out=yo[ob][:, 512:1024], in_=ps[pb][:]), ["ps%d" % pb],
                        ["yo%d" % ob])
            r0 = c * 512 + tt * 128
            dma("sp", out_d[r0:r0 + 128, :], yo[ob][:], "yo%d" % ob, reads=["yo%d" % ob], writes=["out.%d" % (r0 // 128)])
    P.finish()
    P.emit()
    es.close()
    return nc, es


def _col(v):
    v = np.asarray(v, dtype=np.float32)
    return np.ascontiguousarray(v.reshape(-1, 128).T)


def _const_tables():
    cf = np.zeros((128, CONSTF), np.float32)
    cf[:, 0:128] = np.eye(128, dtype=np.float32)
    cf[:, 128:256] = 1.0
    p = np.arange(128)[:, None]
    q = np.arange(512)[None, :]
    for j in range(4):
        cf[:, 256 + j * 512:256 + (j + 1) * 512] = np.where(j * 128 + p > q, -30000.0, 0.0)
    cf[64, 2304:2400] = 1.0
    for e in range(NE):
        cf[e, 2400 + e * 128:2400 + (e + 1) * 128] = 1.0
    half = 16
    inv_freq = (np.float32(10000.0) ** (-np.arange(half, dtype=np.float32) / np.float32(half))).astype(np.float32)
    rope = np.zeros((128, 2), np.float32)
    rope[:, 0] = inv_freq[np.arange(128) % 16]
    rope[:, 1] = np.where((np.arange(128) % 32) < 16, -1.0, 1.0)
    return cf, rope


def prepare_inputs(depth, moe_layers, T, x, c, positions, ada_w, ada_b, attn_norm_g, w_in, q_norm_g, w_uq, kv_norm_g,
                   w_ukv, fox_forget_b, mla_out_g, fox_out_g, w_o, ffn_norm_g, dense_w_gate, dense_w_up, dense_w_down,
                   router_w, moe_w_gate, moe_w_up, moe_w_down, final_norm_g, ncores):
    f32 = lambda a: np.ascontiguousarray(np.asarray(a, dtype=np.float32))
    cf, rope = _const_tables()
    w_in = f32(w_in)[:depth]
    w_uq, w_ukv, ada_w, w_o = f32(w_uq)[:depth], f32(w_ukv)[:depth], f32(ada_w)[:depth], f32(w_o)[:depth]
    cq_, ckv_, kr_, fq_, fk_, fv_, fl_ = (w_in[:, :, 0:256], w_in[:, :, 256:384], w_in[:, :, 384:416],
                                          w_in[:, :, 416:928], w_in[:, :, 928:1440], w_in[:, :, 1440:1952],
                                          w_in[:, :, 1952:1960])
    krb = np.concatenate([kr_[:, :, 16:32], kr_[:, :, 0:16]], axis=-1)
    w_in_p = np.ascontiguousarray(np.concatenate([cq_, ckv_, kr_, ckv_[:, :, 64:128], krb, fl_, fq_, fk_, fv_], axis=-1))
    assert w_in_p.shape[-1] == WIN_COLS
    w_uq = f32(w_uq).reshape(depth, 256, NH, 96)
    wqb = np.concatenate([w_uq[..., 0:64], w_uq[..., 80:96], w_uq[..., 64:80]], axis=-1)
    w_uqa = np.ascontiguousarray(w_uq.reshape(depth, 256, 768))
    w_uqb = np.ascontiguousarray(wqb.reshape(depth, 256, 768))
    wkv = f32(w_ukv).reshape(depth, 128, NH, 128)
    w_ukv_p = np.ascontiguousarray(np.concatenate([wkv[..., 0:64].reshape(depth, 128, 512),
                                                   wkv[..., 64:128].reshape(depth, 128, 512)], axis=-1))
    shared = {
        "constf": cf, "ada_w": f32(ada_w), "w_in": w_in_p, "w_uqa": w_uqa, "w_uqb": w_uqb, "w_ukv": w_ukv_p,
        "w_o": f32(w_o), "dense_w_gate": f32(dense_w_gate), "dense_w_up": f32(dense_w_up),
        "dense_w_down": f32(dense_w_down), "router_w": f32(router_w), "moe_w_gate": f32(moe_w_gate),
        "moe_w_up": f32(moe_w_up), "moe_w_down": f32(moe_w_down),
    }
    in_maps = []
    for b in range(ncores):
        cols = np.zeros((128, NCOL), np.float32)
        for l in range(depth):
            o = l * LCOLS
            cols[:, o:o + 8] = _col(attn_norm_g[l])
            cols[:, o + 8:o + 56] = _col(ada_b[l])
            cols[:, o + 56:o + 58] = _col(q_norm_g[l])
            cols[:, o + 58:o + 59] = _col(kv_norm_g[l])
            cols[:, o + 59:o + 63] = _col(mla_out_g[l])
            cols[:, o + 63:o + 67] = _col(fox_out_g[l])
            cols[:, o + 67:o + 75] = _col(ffn_norm_g[l])
            cols[0:8, 168 + l] = np.asarray(fox_forget_b[l], np.float32)
        cols[:, 150:158] = _col(final_norm_g)
        cols[:, 158:166] = _col(np.asarray(c)[b])
        cols[:, 166:168] = rope
        m = dict(shared)
        m["x"] = f32(np.asarray(x)[b, :T])
        m["posb"] = np.ascontiguousarray(np.broadcast_to(np.asarray(positions)[b, :T].astype(np.int32), (128, T)))
        m["cols"] = cols
        in_maps.append(m)
    return in_maps


_CACHE = {}


def run_config(T, depth, moe_layers, ncores, inputs, debug=False, stop=None):
    key = (T, depth, tuple(moe_layers), debug, stop)
    if key not in _CACHE:
        _CACHE[key] = build_program(T, depth, list(moe_layers), debug=debug, stop=stop)
    nc, _es = _CACHE[key]
    in_maps = prepare_inputs(depth, list(moe_layers), T, ncores=ncores, **inputs)
    res = run_bass_kernel_spmd(nc, in_maps, core_ids=list(range(ncores)))
    out = np.stack([np.asarray(r["out"]) for r in res.results], axis=0)
    if debug:
        return out.astype(np.float32), res.results
    return out.astype(np.float32)


def kernel(**inputs):
    return run_config(4096, 2, [1], 8, inputs)
```

```python
import math
from contextlib import ExitStack

import numpy as np
import concourse.bass as bass
import concourse.mybir as mybir
from concourse.bass_utils import run_bass_kernel_spmd

F32 = mybir.dt.float32
BF16 = mybir.dt.bfloat16
I32 = mybir.dt.int32
AF = mybir.ActivationFunctionType
ALU = mybir.AluOpType
AX = mybir.AxisListType

D = 1024
KC = 8
NH = 8
DFF = 3584
NFG = 7
NE = 8
WIN_COLS = 2056
EPS = 1e-6
NCOL = 170
LCOLS = 75
CONSTF = 128 + 128 + 2048 + 96 + 1024
MLA_SCALE = 96 ** -0.5
TWO_PI = 2.0 * math.pi

DSIZE = {F32: 4, BF16: 2, I32: 4}


class Op:
    __slots__ = ("eng", "fn", "deps", "sem", "ticket", "signal", "is_dma", "idx")


class _Rec:
    def __init__(self):
        self.calls = []

    def __getattr__(self, name):
        def f(*a, **k):
            self.calls.append((name, a, k))
            return None
        return f


class Prog:
    ENGS = ("pe", "act", "dve", "pool", "sp")

    def __init__(self, nc, es):
        self.nc = nc
        self.es = es
        self.ops = {e: [] for e in self.ENGS}
        self.semh = {}
        self.dmacount = {}
        self.lastw = {}
        self.readers = {}
        self.lastdma = {}
        self.pending = {e: [] for e in self.ENGS}
        self.dma_since_bar = []
        self.final_deps = []
        self.n = 0

    def sem(self, name):
        if name not in self.semh:
            self.semh[name] = self.es.enter_context(self.nc.semaphore("s%d" % len(self.semh)))
        return self.semh[name]

    def add(self, eng, fn, reads=(), writes=(), dma_key=None):
        op = Op()
        op.eng = eng
        rec = _Rec()
        fn(rec)
        assert len(rec.calls) == 1
        cname, cargs, ckw = rec.calls[0]
        op.fn = lambda e, _n=cname, _a=cargs, _k=ckw: getattr(e, _n)(*_a, **_k)
        op.is_dma = dma_key is not None
        op.signal = op.is_dma
        op.ticket = None
        op.idx = self.n
        self.n += 1
        deps = {}
        for r in reads:
            w = self.lastw.get(r)
            if w is not None:
                deps[id(w)] = w
        for w_ in writes:
            lw = self.lastw.get(w_)
            if lw is not None:
                deps[id(lw)] = lw
            for rd in self.readers.get(w_, {}).values():
                deps[id(rd)] = rd
        if op.is_dma:
            ld = self.lastdma.get(dma_key)
            if ld is not None:
                deps[id(ld)] = ld
        for p in self.pending[eng]:
            deps[id(p)] = p
        self.pending[eng] = []
        deps.pop(id(op), None)
        if op.is_dma:
            op.sem = "d:" + dma_key
            c = self.dmacount.get(op.sem, 0) + 16
            self.dmacount[op.sem] = c
            op.ticket = c
            self.lastdma[dma_key] = op
            self.dma_since_bar.append(op)
        else:
            op.sem = "e:" + eng
        final = []
        for d in deps.values():
            if (not d.is_dma) and d.eng == "pe" and eng == "pe" and not op.is_dma:
                continue
            d.signal = True
            final.append(d)
        op.deps = final
        rk = op.sem
        for r in reads:
            self.readers.setdefault(r, {})[rk] = op
        for w_ in writes:
            self.lastw[w_] = op
            self.readers[w_] = {}
        self.ops[eng].append(op)
        return op

    def barrier(self):
        deps = []
        for e in self.ENGS:
            if self.ops[e]:
                deps.append(self.ops[e][-1])
        deps.extend(self.dma_since_bar)
        self.dma_since_bar = []
        for d in deps:
            d.signal = True
        for e in self.ENGS:
            self.pending[e] = list(deps)

    def finish(self):
        self.barrier()
        self.final_deps = list(self.pending["sp"])

    def emit(self):
        nc = self.nc
        for e in self.ENGS:
            c = 0
            for op in self.ops[e]:
                if not op.is_dma and op.signal:
                    c += 1
                    op.ticket = c
        handles = {"pe": "tensor", "act": "scalar", "dve": "vector", "pool": "gpsimd", "sp": "sync"}
        import os
        for i_ in range(int(os.environ.get("SEM_SHIFT", "0"))):
            self.sem("dummy%d" % i_)
        names = []
        for e in self.ENGS:
            for op in self.ops[e]:
                if op.signal and op.sem not in names:
                    names.append(op.sem)
        hs = [self.sem(nm) for nm in names]
        with nc.Block() as b0:
            b0.sync(lambda eng: [eng.sem_clear(h) for h in hs])

        def run(ename, eng):
            waited = {}

            def do_waits(deps):
                need = {}
                for d in deps:
                    if d.ticket is None:
                        continue
                    if need.get(d.sem, 0) < d.ticket:
                        need[d.sem] = d.ticket
                for s, v in need.items():
                    if waited.get(s, 0) < v:
                        eng.wait_ge(self.sem(s), v)
                        waited[s] = v

            for op in self.ops[ename]:
                do_waits(op.deps)
                ins = op.fn(eng)
                if op.signal:
                    ins.then_inc(self.sem(op.sem), 16 if op.is_dma else 1)
            if ename == "sp":
                do_waits(self.final_deps)

        with nc.Block() as block:
            for ename in self.ENGS:
                getattr(block, handles[ename])(lambda eng, _n=ename: run(_n, eng))


class Arena:
    def __init__(self, nc, limit):
        self.nc = nc
        self.off = 16512
        self.limit = 16512 + limit
        self.cnt = 0

    def alloc(self, name, shape, dtype):
        n = 1
        for s in shape[1:]:
            n *= s
        nbytes = n * DSIZE[dtype]
        nbytes = (nbytes + 63) // 64 * 64
        assert self.off + nbytes <= self.limit, (name, self.off, nbytes, self.limit)
        self.cnt += 1
        t = self.nc.alloc_sbuf_tensor_at("%s_%d" % (name, self.cnt), list(shape), dtype, offset=self.off)
        self.off += nbytes
        return t

    def mark(self):
        return self.off

    def release(self, m):
        self.off = m


class _Stop(Exception):
    pass


def build_program(T, depth, moe_layers, debug=False, stop=None):
    nc_holder = []
    try:
        return _build_program(T, depth, moe_layers, debug, stop, nc_holder)
    except _Stop:
        nc, es, P = nc_holder
        P.finish()
        P.emit()
        es.close()
        return nc, es


def _build_program(T, depth, moe_layers, debug, stop, nc_holder):
    NCH = T // 512
    NT = T // 128
    nc = bass.Bass("TRN2", target_bir_lowering=False)
    es = ExitStack()
    P = Prog(nc, es)
    nc_holder.extend([nc, es, P])

    def din(name, shape, dt=F32):
        return nc.dram_tensor(name, list(shape), dt, kind="ExternalInput").ap()

    def dscr(name, shape, dt):
        return nc.dram_tensor(name, list(shape), dt, kind=("ExternalOutput" if debug else "Internal")).ap()

    x_d = din("x", [T, D])
    posb_d = din("posb", [128, T], I32)
    cols_d = din("cols", [128, NCOL])
    constf_d = din("constf", [128, CONSTF])
    adaw_d = din("ada_w", [depth, D, 6 * D])
    win_d = din("w_in", [depth, D, WIN_COLS])
    wqa_d = din("w_uqa", [depth, 256, 768])
    wqb_d = din("w_uqb", [depth, 256, 768])
    wkv_d = din("w_ukv", [depth, 128, 1024])
    wo_d = din("w_o", [depth, D, D])
    n_moe = max(1, len(moe_layers))
    n_dense = max(1, depth - len(moe_layers))
    dg_d = din("dense_w_gate", [n_dense, D, DFF])
    du_d = din("dense_w_up", [n_dense, D, DFF])
    dd_d = din("dense_w_down", [n_dense, DFF, D])
    rw_d = din("router_w", [n_moe, D, NE])
    mg_d = din("moe_w_gate", [n_moe, NE, D, DFF])
    mu_d = din("moe_w_up", [n_moe, NE, D, DFF])
    md_d = din("moe_w_down", [n_moe, NE, DFF, D])
    out_d = nc.dram_tensor("out", [T, D], F32, kind="ExternalOutput").ap()

    xT_d = dscr("xT", [KC, 128, T], F32)
    QM_d = dscr("QM", [NH, 96, T], BF16)
    KM_d = dscr("KM", [NH, 96, T], BF16)
    VM_d = dscr("VM", [NH, 128, NT, 65], BF16)
    QF_d = dscr("QF", [4, 128, T], BF16)
    KF_d = dscr("KF", [4, 128, T], BF16)
    QA_d = dscr("QA", [NH, 6, T], BF16)
    KA_d = dscr("KA", [NH, 6, T], BF16)
    VF_d = dscr("VF", [NH, 128, NT, 65], BF16)
    OT_d = dscr("OT", [KC, 128, T], BF16)
    H2_d = dscr("H2", [KC, 128, T], BF16)
    GB_d = dscr("GB", [NE, 128, T], F32)

    A = Arena(nc, 206 * 1024)
    ps = [nc.alloc_psum_tensor("ps%d" % i, [128, 512], F32) for i in range(8)]

    colsb = A.alloc("cols", [128, NCOL], F32)
    identf = A.alloc("identf", [128, 128], F32)
    sell = A.alloc("sell", [128, 96], F32)
    sele = A.alloc("sele", [128, NE, 128], F32)
    identb = A.alloc("identb", [128, 128], BF16)
    onesb = A.alloc("onesb", [128, 128], BF16)
    maskb = A.alloc("maskb", [128, 4, 512], BF16)
    modsb = [A.alloc("mod%d" % l, [128, 48], F32) for l in range(depth)]
    gsa = [A.alloc("gsa%d" % l, [128, 8], F32) for l in range(depth)]
    gsf = [A.alloc("gsf%d" % l, [128, 8], F32) for l in range(depth)]
    cact = A.alloc("cact", [128, 8], F32)
    negfb = A.alloc("negfb", [128, 2], F32)
    sgn2pi = A.alloc("sgn2pi", [128, 1], F32)

    def dma(q, out, in_, key, reads=(), writes=()):
        return P.add(q, lambda e, o=out, i=in_: e.dma_start(out=o, in_=i), reads=reads, writes=writes, dma_key=key)

    def mm(out, lhsT, rhs, start, stop, reads, writes):
        return P.add("pe", lambda e, o=out, l=lhsT, r=rhs, s=start, t=stop: e.matmul(o, l, r, start=s, stop=t),
                     reads=reads, writes=writes)

    def act(out, in_, func, reads, writes, bias=None, scale=None):
        kw = {}
        if bias is not None:
            kw["bias"] = bias
        if scale is not None:
            kw["scale"] = scale
        return P.add("act", lambda e, o=out, i=in_, f=func, k=kw: e.activation(out=o, in_=i, func=f, **k),
                     reads=reads, writes=writes)

    def dve(fn, reads, writes, eng="dve"):
        return P.add(eng, fn, reads=reads, writes=writes)

    def dbg(name, ap, shape, dt, keys):
        if not debug:
            return
        d_ = nc.dram_tensor("dbg_" + name, list(shape), dt, kind="ExternalOutput").ap()
        dma("sp", d_, ap, "dbg_" + name, reads=keys, writes=["dbg_" + name])

    dma("sp", colsb[:], cols_d, "cols", writes=["cols"])
    dma("sp", identf[:], constf_d[:, 0:128], "identf", writes=["identf"])
    dma("sp", sell[:], constf_d[:, 2304:2400], "sell", writes=["sell"])
    dma("sp", sele[:], constf_d[:, 2400:3424].rearrange("p (e m) -> p e m", e=NE), "sele", writes=["sele"])
    dma("pool", identb[:], constf_d[:, 0:128], "identb", writes=["identb"])
    dma("pool", onesb[:], constf_d[:, 128:256], "onesb", writes=["onesb"])
    dma("pool", maskb[:], constf_d[:, 256:2304].rearrange("p (j q) -> p j q", j=4), "maskb", writes=["maskb"])

    act(cact[:], colsb[:, 158:166], AF.Silu, ["cols"], ["cact"])
    dve(lambda e: e.tensor_scalar(out=negfb[:], in0=colsb[:, 168:170], scalar1=-1.0, scalar2=None, op0=ALU.mult),
        ["cols"], ["negfb"])
    dve(lambda e: e.tensor_scalar(out=sgn2pi[:], in0=colsb[:, 167:168], scalar1=TWO_PI * (1.0 - 2e-6), scalar2=None,
                                  op0=ALU.mult), ["cols"], ["sgn2pi"])

    m0 = A.mark()
    awb = [A.alloc("aw%d" % i, [128, KC, 512], F32) for i in range(2)]
    for l in range(depth):
        for jg in range(12):
            b = (l * 12 + jg) % 2
            dma("sp", awb[b][:], adaw_d[l, :, jg * 512:(jg + 1) * 512].rearrange("(k p) n -> p k n", p=128),
                "aw%d" % b, writes=["aw%d" % b])
            for jj in range(4):
                j = jg * 4 + jj
                for k in range(KC):
                    mm(ps[0][:, j:j + 1], awb[b][:, k, jj * 128:(jj + 1) * 128], cact[:, k:k + 1],
                       k == 0, k == KC - 1, ["aw%d" % b, "cact"], ["ps0"])
        c0 = l * LCOLS
        dve(lambda e, l=l, c0=c0: e.tensor_tensor(out=modsb[l][:], in0=ps[0][:, 0:48], in1=colsb[:, c0 + 8:c0 + 56],
                                                  op=ALU.add), ["ps0", "cols"], ["mod%d" % l])
        dve(lambda e, l=l, c0=c0: e.scalar_tensor_tensor(out=gsa[l][:], in0=modsb[l][:, 8:16], scalar=1.0,
                                                         in1=colsb[:, c0:c0 + 8], op0=ALU.add, op1=ALU.mult),
            ["mod%d" % l, "cols"], ["gsa%d" % l])
        dve(lambda e, l=l, c0=c0: e.scalar_tensor_tensor(out=gsf[l][:], in0=modsb[l][:, 32:40], scalar=1.0,
                                                         in1=colsb[:, c0 + 67:c0 + 75], op0=ALU.add, op1=ALU.mult),
            ["mod%d" % l, "cols"], ["gsf%d" % l])
    dbg("mod0", modsb[0][:], [128, 48], F32, ["mod0"])
    dbg("gsa0", gsa[0][:], [128, 8], F32, ["gsa0"])
    dbg("cact", cact[:], [128, 8], F32, ["cact"])
    dbg("cols", colsb[:], [128, NCOL], F32, ["cols"])
    dbg("onesb", onesb[:], [128, 128], BF16, ["onesb"])
    P.barrier()
    A.release(m0)

    def rstd_from(psb, pskey, rs, rskey, n):
        act(rs, psb, AF.Sqrt, [pskey], [rskey], bias=EPS, scale=1.0 / n)
        dve(lambda e, rs=rs: e.reciprocal(out=rs, in_=rs), [rskey], [rskey])

    for l in range(depth):
        c0 = l * LCOLS
        is_moe = l in moe_layers
        m1 = A.mark()
        wi = A.alloc("wi", [128, KC, WIN_COLS], BF16)
        wqa = A.alloc("wqa", [128, 2, 768], BF16)
        wqb = A.alloc("wqb", [128, 2, 768], BF16)
        wkv = A.alloc("wkv", [128, 1024], BF16)
        for k in range(KC):
            dma("pool", wi[:, k, :], win_d[l, k * 128:(k + 1) * 128, :], "wi%d" % k, writes=["wi%d" % k])
        WI = ["wi%d" % k for k in range(KC)]
        dma("pool", wqa[:], wqa_d[l].rearrange("(j p) n -> p j n", p=128), "wqa", writes=["wqa"])
        dma("pool", wqb[:], wqb_d[l].rearrange("(j p) n -> p j n", p=128), "wqb", writes=["wqb"])
        dma("pool", wkv[:], wkv_d[l], "wkv", writes=["wkv"])
        xin = A.alloc("xin", [128, 4, D], F32) if l == 0 else None
        xt = A.alloc("xt", [128, KC, 512], F32)
        sq = A.alloc("sq", [128, KC, 512], BF16)
        tmp = A.alloc("tmp", [128, 2, 512], F32)
        hT = A.alloc("hT", [128, KC, 512], BF16)
        rs = A.alloc("rs", [128, 512], F32)
        cq = A.alloc("cq", [128, 2, 512], F32)
        sqq = A.alloc("sqq", [128, 2, 512], BF16)
        rsq = A.alloc("rsq", [128, 512], F32)
        qn = A.alloc("qn", [128, 2, 512], BF16)
        ckv = A.alloc("ckv", [128, 512], F32)
        sqk = A.alloc("sqk", [128, 512], BF16)
        rsk = A.alloc("rsk", [128, 512], F32)
        kvn = A.alloc("kvn", [128, 512], BF16)
        qt = A.alloc("qt", [96, NH, 512], BF16)
        kt_ = A.alloc("kt", [96, NH, 512], BF16)
        krope = A.alloc("krope", [96, 512], BF16)
        r1 = A.alloc("r1", [96, 512], F32)
        r2 = A.alloc("r2", [96, 512], F32)
        vm = A.alloc("vm", [128, NH, 4, 65], BF16)
        vf = A.alloc("vf", [128, NH, 4, 65], BF16)
        fq = A.alloc("fq", [128, 4, 512], BF16)
        fk = A.alloc("fk", [128, 4, 512], BF16)
        posi = A.alloc("posi", [128, 512], I32)
        rr = A.alloc("rr", [128, 512], F32)
        ri = A.alloc("ri", [128, 512], I32)
        rf = A.alloc("rf", [128, 512], F32)
        ff = A.alloc("ff", [128, 512], F32)
        gg = A.alloc("gg", [128, 512], F32)
        cosb = A.alloc("cosb", [128, 512], F32)
        sinb = A.alloc("sinb", [128, 512], F32)
        ez = A.alloc("ez", [8, 512], F32)
        lp = A.alloc("lp", [8, 512], F32)
        ones8 = A.alloc("ones8", [8, 512], F32)
        Fc = [A.alloc("Fc%d" % i, [8, 512], F32) for i in range(2)]
        fr = A.alloc("fr", [8, 512], F32)
        fb = A.alloc("fb", [8, 512], BF16)
        augq = A.alloc("augq", [8, 6, 512], BF16)
        augk = A.alloc("augk", [8, 6, 512], BF16)

        dve(lambda e: e.memset(vm[:], 1.0), [], ["vm"])
        dve(lambda e: e.memset(vf[:], 1.0), [], ["vf"])
        dve(lambda e: e.memset(ones8[:], 1.0), [], ["ones8"])
        dve(lambda e: e.memset(augq[:], 1.0), [], ["augq"])
        dve(lambda e: e.memset(augk[:], 1.0), [], ["augk"])

        for c in range(NCH):
            cs = slice(c * 512, (c + 1) * 512)
            def p1_loads(cc):
                cs_ = slice(cc * 512, (cc + 1) * 512)
                if l == 0:
                    dma("sp", xin[:], x_d[cs_, :].rearrange("(t p) d -> p t d", p=128), "xin", writes=["xin"])
                else:
                    dma("sp", xt[:], xT_d[:, :, cs_].rearrange("k p t -> p k t"), "xt", reads=["xT.%d" % cc],
                        writes=["xt"])
                dma("sp", posi[:], posb_d[:, cs_], "posi", writes=["posi"])
            if c == 0:
                p1_loads(0)
            if l == 0:
                for k in range(KC):
                    pb = k % 2
                    for tt in range(4):
                        mm(ps[pb][:, tt * 128:(tt + 1) * 128], xin[:, tt, k * 128:(k + 1) * 128], identf[:], True, True,
                           ["xin", "identf"], ["ps%d" % pb])
                    act(xt[:, k, :], ps[pb][:], AF.Copy, ["ps%d" % pb], ["xt"])
                dma("sp", xT_d[:, :, cs].rearrange("k p t -> p k t"), xt[:], "xt", reads=["xt"], writes=["xT.%d" % c])
            act(sq[:], xt[:], AF.Square, ["xt"], ["sq"])
            for k in range(KC):
                mm(ps[2][:], onesb[:], sq[:, k, :], k == 0, k == KC - 1, ["onesb", "sq"], ["ps2"])
            rstd_from(ps[2][:], "ps2", rs[:], "rs", D)
            for k in range(KC):
                dve(lambda e, k=k: e.tensor_tensor(out=tmp[:, k % 2, :], in0=xt[:, k, :], in1=rs[:], op=ALU.mult),
                    ["xt", "rs"], ["tmp%d" % (k % 2)])
                act(hT[:, k, :], tmp[:, k % 2, :], AF.Identity, ["tmp%d" % (k % 2), "gsa%d" % l, "mod%d" % l], ["hT%d" % k],
                    bias=modsb[l][:, k:k + 1], scale=gsa[l][:, k:k + 1])
            HT = ["hT%d" % k for k in range(KC)]
            if c == 0 and l == 0:
                dbg("xt", xt[:], [128, KC, 512], F32, ["xt"])
                if stop == "xt":
                    raise _Stop()
                dbg("sq", sq[:], [128, KC, 512], BF16, ["sq"])
                dbg("rs", rs[:], [128, 512], F32, ["rs"])
                dbg("hT", hT[:], [128, KC, 512], BF16, HT)
                dbg("wi", wi[:], [128, KC, WIN_COLS], BF16, WI)
                if stop == "hT":
                    raise _Stop()

            def proj(pb, col0, ncols, extra_reads=()):
                for k in range(KC):
                    mm(ps[pb][0:ncols, :], wi[:, k, col0:col0 + ncols], hT[:, k, :], k == 0, k == KC - 1,
                       [WI[k], HT[k]] + list(extra_reads), ["ps%d" % pb])

            dve(lambda e: e.tensor_copy(out=rr[:], in_=posi[:]), ["posi"], ["rr"])
            dve(lambda e: e.tensor_scalar(out=rr[:], in0=rr[:], scalar1=colsb[:, 166:167], scalar2=1.0 / TWO_PI,
                                          op0=ALU.mult, op1=ALU.mult), ["rr", "cols"], ["rr"])
            for which in range(2):
                dst = sinb if which == 0 else cosb
                dkey = "sinb" if which == 0 else "cosb"
                if which == 1:
                    dve(lambda e: e.tensor_scalar(out=rr[:], in0=rr[:], scalar1=0.25, scalar2=None, op0=ALU.add),
                        ["rr"], ["rr"])
                dve(lambda e: e.tensor_copy(out=ri[:], in_=rr[:]), ["rr"], ["ri"])
                dve(lambda e: e.tensor_copy(out=rf[:], in_=ri[:]), ["ri"], ["rf"])
                dve(lambda e: e.tensor_tensor(out=ff[:], in0=rr[:], in1=rf[:], op=ALU.subtract), ["rr", "rf"], ["ff"])
                dve(lambda e: e.tensor_scalar(out=gg[:], in0=ff[:], scalar1=0.5, scalar2=None, op0=ALU.is_gt),
                    ["ff"], ["gg"])
                dve(lambda e: e.tensor_tensor(out=ff[:], in0=ff[:], in1=gg[:], op=ALU.subtract), ["ff", "gg"], ["ff"])
                dve(lambda e: e.tensor_scalar(out=gg[:], in0=ff[:], scalar1=-0.5, scalar2=None, op0=ALU.is_lt),
                    ["ff"], ["gg"])
                dve(lambda e: e.tensor_tensor(out=ff[:], in0=ff[:], in1=gg[:], op=ALU.add), ["ff", "gg"], ["ff"])
                if which == 0:
                    act(dst[:], ff[:], AF.Sin, ["ff", "sgn2pi"], [dkey], scale=sgn2pi[:, 0:1])
                else:
                    act(dst[:], ff[:], AF.Sin, ["ff"], [dkey], scale=TWO_PI * (1.0 - 2e-6))

            if c + 1 < NCH:
                p1_loads(c + 1)

            def rope_rows(psa, psb, pka, pkb, dst, dkey):
                dve(lambda e: e.tensor_tensor(out=r1[64:96, :], in0=psa[64:96, :], in1=cosb[64:96, :], op=ALU.mult),
                    [pka, "cosb"], ["r1"])
                dve(lambda e: e.tensor_tensor(out=r2[64:96, :], in0=psb[64:96, :], in1=sinb[64:96, :], op=ALU.mult),
                    [pkb, "sinb"], ["r2"])
                dve(lambda e, dst=dst: e.tensor_tensor(out=dst, in0=r1[64:96, :], in1=r2[64:96, :], op=ALU.add),
                    ["r1", "r2"], [dkey])

            if stop == "rope":
                raise _Stop()
            for j in range(2):
                proj(3 + j, j * 128, 128)
                act(cq[:, j, :], ps[3 + j][:], AF.Copy, ["ps%d" % (3 + j)], ["cq%d" % j])
            if stop == "cq":
                raise _Stop()
            proj(5, 256, 128)
            act(ckv[:], ps[5][:], AF.Copy, ["ps5"], ["ckv"])
            if stop == "ckv":
                raise _Stop()
            proj(6, 320, 96)
            proj(7, 416, 96)
            rope_rows(ps[6], ps[7], "ps6", "ps7", krope[64:96, :], "krope")
            if stop == "krope":
                raise _Stop()
            proj(3, 512, 128)
            act(ez[:], ps[3][0:8, :], AF.Exp, ["ps3", "negfb"], ["ez"], bias=negfb[0:8, l:l + 1], scale=-1.0)
            act(lp[:], ez[:], AF.Ln, ["ez"], ["lp"], bias=1.0)
            cur, prev = Fc[c % 2], Fc[(c + 1) % 2]
            init = 0.0 if c == 0 else prev[:, 511:512]
            dve(lambda e, cur=cur, init=init: e.tensor_tensor_scan(out=cur[:], data0=ones8[:], data1=lp[:], initial=init,
                                                                   op0=ALU.mult, op1=ALU.subtract),
                ["ones8", "lp", "Fc%d" % ((c + 1) % 2)], ["Fc%d" % (c % 2)])
            fkey = "Fc%d" % (c % 2)
            dve(lambda e, cur=cur: e.tensor_copy(out=augq[:, 0, :], in_=cur[:]), [fkey], ["augq"])
            dve(lambda e, cur=cur: e.tensor_tensor(out=fr[:], in0=cur[:], in1=augq[:, 0, :], op=ALU.subtract),
                [fkey, "augq"], ["fr"])
            dve(lambda e: e.tensor_copy(out=augq[:, 1, :], in_=fr[:]), ["fr"], ["augq"])
            dve(lambda e: e.tensor_tensor(out=fr[:], in0=fr[:], in1=augq[:, 1, :], op=ALU.subtract), ["fr", "augq"], ["fr"])
            dve(lambda e: e.tensor_copy(out=augq[:, 2, :], in_=fr[:]), ["fr"], ["augq"])
            for i3 in range(3):
                dve(lambda e, i3=i3: e.tensor_scalar(out=augk[:, 3 + i3, :], in0=augq[:, i3, :], scalar1=-1.0,
                                                     scalar2=None, op0=ALU.mult), ["augq"], ["augk"])
            dma("sp", QA_d[:, :, cs], augq[:], "augq", reads=["augq"], writes=["QA.%d" % c])
            dma("sp", KA_d[:, :, cs], augk[:], "augk", reads=["augk"], writes=["KA.%d" % c])
            if stop == "gates":
                raise _Stop()
            for j in range(4):
                pb = 4 + (j % 2)
                proj(pb, 520 + j * 128, 128)
                act(fq[:, j, :], ps[pb][:], AF.Copy, ["ps%d" % pb], ["fq"], scale=0.125)
            dma("sp", QF_d[:, :, cs].rearrange("j p t -> p j t"), fq[:], "fq", reads=["fq"], writes=["QF.%d" % c])
            for j in range(4):
                pb = 6 + (j % 2)
                proj(pb, 1032 + j * 128, 128)
                dve(lambda e, j=j, pb=pb: e.tensor_copy(out=fk[:, j, :], in_=ps[pb][:]), ["ps%d" % pb], ["fk"])
            dma("sp", KF_d[:, :, cs].rearrange("j p t -> p j t"), fk[:], "fk", reads=["fk"], writes=["KF.%d" % c])
            if stop == "fqk":
                raise _Stop()
            for tt in range(4):
                pb = 3 + (tt % 2)
                for k in range(KC):
                    mm(ps[pb][:], hT[:, k, tt * 128:(tt + 1) * 128], wi[:, k, 1544:2056], k == 0, k == KC - 1,
                       [HT[k], WI[k]], ["ps%d" % pb])
                act(vf[:, :, tt, 0:64], ps[pb][:].rearrange("p (h d) -> p h d", h=NH), AF.Copy, ["ps%d" % pb], ["vf"])
            dma("sp", VF_d[:, :, c * 4:(c + 1) * 4, :].rearrange("h p t d -> p h t d"), vf[:], "vf", reads=["vf"],
                writes=["VF.%d" % c])
            if stop == "fv":
                raise _Stop()
            act(sqq[:], cq[:], AF.Square, ["cq0", "cq1"], ["sqq"])
            for j in range(2):
                mm(ps[2][:], onesb[:], sqq[:, j, :], j == 0, j == 1, ["onesb", "sqq"], ["ps2"])
            rstd_from(ps[2][:], "ps2", rsq[:], "rsq", 256)
            for j in range(2):
                dve(lambda e, j=j: e.scalar_tensor_tensor(out=qn[:, j, :], in0=cq[:, j, :],
                                                          scalar=colsb[:, c0 + 56 + j:c0 + 57 + j], in1=rsq[:],
                                                          op0=ALU.mult, op1=ALU.mult),
                    ["cq%d" % j, "rsq", "cols"], ["qn"])
            for h in range(NH):
                pa, pbb = (4, 5) if h % 2 == 0 else (6, 7)
                for j in range(2):
                    mm(ps[pa][0:96, :], wqa[:, j, h * 96:(h + 1) * 96], qn[:, j, :], j == 0, j == 1, ["wqa", "qn"],
                       ["ps%d" % pa])
                for j in range(2):
                    mm(ps[pbb][0:96, :], wqb[:, j, h * 96:(h + 1) * 96], qn[:, j, :], j == 0, j == 1, ["wqb", "qn"],
                       ["ps%d" % pbb])
                act(qt[0:64, h, :], ps[pa][0:64, :], AF.Copy, ["ps%d" % pa], ["qt.n"])
                rope_rows(ps[pa], ps[pbb], "ps%d" % pa, "ps%d" % pbb, qt[64:96, h, :], "qt.r")
            dma("sp", QM_d[:, :, cs].rearrange("h r t -> r h t"), qt[:], "qt", reads=["qt.n", "qt.r"], writes=["QM.%d" % c])
            if stop == "qpath":
                raise _Stop()
            act(sqk[:], ckv[:], AF.Square, ["ckv"], ["sqk"])
            mm(ps[2][:], onesb[:], sqk[:], True, True, ["onesb", "sqk"], ["ps2"])
            rstd_from(ps[2][:], "ps2", rsk[:], "rsk", 128)
            dve(lambda e: e.scalar_tensor_tensor(out=kvn[:], in0=ckv[:], scalar=colsb[:, c0 + 58:c0 + 59], in1=rsk[:],
                                                 op0=ALU.mult, op1=ALU.mult), ["ckv", "rsk", "cols"], ["kvn"])
            for h in range(NH):
                pb = 3 + (h % 2)
                mm(ps[pb][0:96, :], wkv[:, h * 64:h * 64 + 96], kvn[:], True, True, ["wkv", "kvn"], ["ps%d" % pb])
                act(kt_[0:64, h, :], ps[pb][0:64, :], AF.Copy, ["ps%d" % pb], ["kt.n"])
                P.add("pool", lambda e, h=h: e.tensor_copy(out=kt_[64:96, h, :], in_=krope[64:96, :]),
                      reads=["krope"], writes=["kt.r"])
            dma("sp", KM_d[:, :, cs].rearrange("h r t -> r h t"), kt_[:], "kt", reads=["kt.n", "kt.r"], writes=["KM.%d" % c])
            for tt in range(4):
                pb = 4 + (tt % 2)
                mm(ps[pb][:], kvn[:, tt * 128:(tt + 1) * 128], wkv[:, 512:1024], True, True, ["kvn", "wkv"],
                   ["ps%d" % pb])
                dve(lambda e, tt=tt, pb=pb: e.tensor_copy(out=vm[:, :, tt, 0:64],
                                                          in_=ps[pb][:].rearrange("p (h d) -> p h d", h=NH)),
                    ["ps%d" % pb], ["vm"])
            dma("sp", VM_d[:, :, c * 4:(c + 1) * 4, :].rearrange("h p t d -> p h t d"), vm[:], "vm", reads=["vm"],
                writes=["VM.%d" % c])
        if stop == "p1":
            raise _Stop()
        P.barrier()
        A.release(m1)

        m2 = A.mark()
        Kb = [A.alloc("Kb%d" % i, [96, T], BF16) for i in range(2)]
        Qb = [A.alloc("Qb%d" % i, [96, T], BF16) for i in range(2)]
        Vb = [A.alloc("Vb%d" % i, [128, NT, 65], BF16) for i in range(2)]
        Ob = [A.alloc("Ob%d" % i, [64, T], BF16) for i in range(2)]
        NPT = 5
        Pt = [A.alloc("Pt%d" % i, [128, 512], BF16) for i in range(NPT)]
        Osb = [A.alloc("Osb%d" % i, [65, 512], F32) for i in range(2)]
        ALLC = lambda pre: [pre + ".%d" % c for c in range(NCH)]
        LA = 3
        items = []
        hh = 0
        gcount = 0
        for typ in range(2):
            for h in range(NH):
                for qc in range(NCH):
                    nk = 4 * (qc + 1)
                    for kt in range(nk):
                        items.append(dict(typ=typ, h=h, hh=hh, qc=qc, kt=kt, nk=nk, g=gcount))
                    gcount += 1
                hh += 1
        loaded = set()

        def load_head(it):
            if it["hh"] in loaded:
                return
            loaded.add(it["hh"])
            typ, h, b = it["typ"], it["h"], it["hh"] % 2
            kk, qk, vk = "Kb%d" % b, "Qb%d" % b, "Vb%d" % b
            if typ == 0:
                dma("sp", Kb[b][0:96, :], KM_d[h], kk, reads=ALLC("KM"), writes=[kk + "m", kk + "a"])
                dma("sp", Qb[b][0:96, :], QM_d[h], qk, reads=ALLC("QM"), writes=[qk + "m", qk + "a"])
                dma("sp", Vb[b][:], VM_d[h], vk, reads=ALLC("VM"), writes=[vk])
            else:
                hp, ho = h // 2, (h % 2) * 64
                dma("sp", Kb[b][0:64, :], KF_d[hp, ho:ho + 64, :], kk, reads=ALLC("KF"), writes=[kk + "m"])
                dma("sp", Kb[b][64:70, :], KA_d[h], kk + "a", reads=ALLC("KA"), writes=[kk + "a"])
                dma("sp", Qb[b][0:64, :], QF_d[hp, ho:ho + 64, :], qk, reads=ALLC("QF"), writes=[qk + "m"])
                dma("sp", Qb[b][64:70, :], QA_d[h], qk + "a", reads=ALLC("QA"), writes=[qk + "a"])
                dma("sp", Vb[b][:], VF_d[h], vk, reads=ALLC("VF"), writes=[vk])

        def emit_qk(n, it):
            typ, b, qc, kt = it["typ"], it["hh"] % 2, it["qc"], it["kt"]
            R = 96 if typ == 0 else 70
            j = kt - 4 * qc
            sb, pb = n % 4, n % NPT
            skey = "ps%d" % sb
            kr = ["Kb%dm" % b, "Kb%da" % b]
            qr = ["Qb%dm" % b, "Qb%da" % b]
            c0_ = max(j, 0) * 128
            mm(ps[sb][:, c0_:512], Kb[b][0:R, kt * 128:(kt + 1) * 128], Qb[b][0:R, qc * 512 + c0_:(qc + 1) * 512],
               True, j < 0, kr + qr, [skey])
            if j >= 0:
                mm(ps[sb][:, c0_:c0_ + 128], identb[:], maskb[:, 0, 0:128], False, True, ["identb", "maskb"], [skey])
            act(Pt[pb][:, c0_:512], ps[sb][:, c0_:512], AF.Exp, [skey], ["Pt%d" % pb],
                scale=(MLA_SCALE if typ == 0 else 1.0))

        def emit_pv(n, it):
            b, kt, nk, ob = it["hh"] % 2, it["kt"], it["nk"], it["g"] % 2
            pb = n % NPT
            c0_ = max(kt - 4 * it["qc"], 0) * 128
            mm(ps[4 + ob][0:65, c0_:512], Vb[b][:, kt, :], Pt[pb][:, c0_:512], kt == 0, kt == nk - 1,
               ["Vb%d" % b, "Pt%d" % pb], ["ps%d" % (4 + ob)])

        def epi1(it):
            ob = it["g"] % 2
            dve(lambda e: e.tensor_copy(out=Osb[ob][:], in_=ps[4 + ob][0:65, :]), ["ps%d" % (4 + ob)], ["Osb%d" % ob])
            dve(lambda e: e.reciprocal(out=Osb[ob][64:65, :], in_=Osb[ob][64:65, :]), ["Osb%d" % ob], ["Osb%d" % ob])

        def epi2(it):
            ob, b, qc, typ, h = it["g"] % 2, it["hh"] % 2, it["qc"], it["typ"], it["h"]
            ok = "Ob%d" % b
            mm(ps[6 + ob][0:96, :], sell[0:65, :], Osb[ob][:], True, True, ["sell", "Osb%d" % ob], ["ps%d" % (6 + ob)])
            dve(lambda e: e.tensor_tensor(out=Ob[b][:, qc * 512:(qc + 1) * 512], in0=Osb[ob][0:64, :],
                                          in1=ps[6 + ob][0:64, :], op=ALU.mult),
                ["Osb%d" % ob, "ps%d" % (6 + ob)], [ok])
            if qc == NCH - 1:
                kch = typ * 4 + h // 2
                ho = (h % 2) * 64
                dma("sp", OT_d[kch, ho:ho + 64, :], Ob[b][:], ok, reads=[ok], writes=["OT.%d.%d" % (kch, h % 2)])

        NI = len(items)
        deferred = []
        for n in range(NI + LA):
            if n < NI:
                load_head(items[n])
                emit_qk(n, items[n])
            m_ = n - LA
            if m_ >= 0:
                it = items[m_]
                if it["kt"] == 0 and it["qc"] == 0:
                    nxt = [x for x in items[m_:m_ + 40 * NCH * NCH + 8] if x["hh"] == it["hh"] + 1]
                    if nxt:
                        load_head(nxt[0])
                emit_pv(m_, it)
                if it["kt"] == it["nk"] - 1:
                    epi1(it)
                    deferred.append((n + 2, it))
            while deferred and (deferred[0][0] <= n or n == NI + LA - 1):
                epi2(deferred.pop(0)[1])
        if stop == "p2":
            raise _Stop()
        P.barrier()
        A.release(m2)

        m3 = A.mark()
        wo = A.alloc("wo", [128, KC, D], BF16)
        dma("pool", wo[:], wo_d[l].rearrange("(k p) n -> p k n", p=128), "wo", writes=["wo"])
        ot = [A.alloc("ot%d" % i_, [128, KC, 512], BF16) for i_ in range(2)]
        sqo = [A.alloc("sqo%d" % i_, [128, KC, 512], BF16) for i_ in range(2)]
        on = [A.alloc("on%d" % i_, [128, KC, 512], BF16) for i_ in range(2)]
        xt = [A.alloc("xt3%d" % i_, [128, KC, 512], F32) for i_ in range(2)]
        tmp = A.alloc("tmp3", [128, 2, 512], F32)
        h2 = [A.alloc("h2%d" % i_, [128, KC, 512], BF16) for i_ in range(2)]
        rsm = A.alloc("rsm", [128, 512], F32)
        rsf = A.alloc("rsf", [128, 512], F32)
        rs2 = A.alloc("rs2", [128, 512], F32)
        if is_moe:
            mi = moe_layers.index(l)
            h2f = [A.alloc("h2f%d" % i_, [128, KC, 512], F32) for i_ in range(2)]
            rw = A.alloc("rw", [128, KC, NE], F32)
            dma("sp", rw[:], rw_d[mi].rearrange("(k p) e -> p k e", p=128), "rw", writes=["rw"])
            lg = A.alloc("lg", [128, 4, NE], F32)
            m1t = A.alloc("m1t", [128, 4], F32)
            nm1 = A.alloc("nm1", [128, 4], F32)
            m2t = A.alloc("m2t", [128, 4], F32)
            eq = A.alloc("eq", [128, 4, NE], F32)
            mk = A.alloc("mk", [128, 4, NE], F32)
            selt = A.alloc("selt", [128, 4, NE], F32)
            ex = A.alloc("ex", [128, 4, NE], F32)
            den = A.alloc("den", [128, 4], F32)
            gt = A.alloc("gt", [128, 4, 128], F32)
            gT = A.alloc("gT", [128, 512], F32)
            gb = A.alloc("gb", [128, NE, 512], F32)
            dve(lambda e: e.memset(gt[:], 0.0), [], ["gt"])
        ALLO = ["OT.%d.%d" % (k, s) for k in range(KC) for s in range(2)]
        for c in range(NCH):
            p_ = c % 2
            cs = slice(c * 512, (c + 1) * 512)
            def p3_loads(cc):
                q_ = cc % 2
                cs_ = slice(cc * 512, (cc + 1) * 512)
                dma("sp", ot[q_][:], OT_d[:, :, cs_].rearrange("k p t -> p k t"), "ot%d" % q_, reads=ALLO,
                    writes=["ot%d" % q_])
                dma("sp", xt[q_][:], xT_d[:, :, cs_].rearrange("k p t -> p k t"), "xt3%d" % q_, reads=["xT.%d" % cc],
                    writes=["xt3%d" % q_])
            if c == 0:
                p3_loads(0)
            if c + 1 < NCH:
                p3_loads(c + 1)
            if stop == "p3a":
                raise _Stop()
            act(sqo[p_][:], ot[p_][:], AF.Square, ["ot%d" % p_], ["sqo%d" % p_])
            for k in range(4):
                mm(ps[0][:], onesb[:], sqo[p_][:, k, :], k == 0, k == 3, ["onesb", "sqo%d" % p_], ["ps0"])
            for k in range(4):
                mm(ps[1][:], onesb[:], sqo[p_][:, 4 + k, :], k == 0, k == 3, ["onesb", "sqo%d" % p_], ["ps1"])
            if stop == "p3b":
                raise _Stop()
            rstd_from(ps[0][:], "ps0", rsm[:], "rsm", 512)
            rstd_from(ps[1][:], "ps1", rsf[:], "rsf", 512)
            for k in range(KC):
                rsx, rkey = (rsm, "rsm") if k < 4 else (rsf, "rsf")
                gcol = c0 + 59 + k
                dve(lambda e, k=k, rsx=rsx, gcol=gcol: e.scalar_tensor_tensor(
                    out=on[p_][:, k, :], in0=ot[p_][:, k, :], scalar=colsb[:, gcol:gcol + 1], in1=rsx[:], op0=ALU.mult,
                    op1=ALU.mult), ["ot%d" % p_, rkey, "cols"], ["on%d_%d" % (p_, k)])
            if stop == "p3c":
                raise _Stop()
            for i in range(KC):
                pb = 2 + (i % 2)
                for k in range(KC):
                    mm(ps[pb][:], wo[:, k, i * 128:(i + 1) * 128], on[p_][:, k, :], k == 0, k == KC - 1,
                       ["wo", "on%d_%d" % (p_, k)], ["ps%d" % pb])
                dve(lambda e, i=i, pb=pb: e.scalar_tensor_tensor(out=xt[p_][:, i, :], in0=ps[pb][:],
                                                                 scalar=modsb[l][:, 16 + i:17 + i], in1=xt[p_][:, i, :],
                                                                 op0=ALU.mult, op1=ALU.add),
                    ["ps%d" % pb, "mod%d" % l, "xt3%d" % p_], ["xt3%d" % p_])
            dma("sp", xT_d[:, :, cs].rearrange("k p t -> p k t"), xt[p_][:], "xt3%d" % p_, reads=["xt3%d" % p_], writes=["xT.%d" % c])
            if stop == "p3e":
                raise _Stop()
            act(sqo[p_][:], xt[p_][:], AF.Square, ["xt3%d" % p_], ["sqo%d" % p_])
            for k in range(KC):
                mm(ps[4][:], onesb[:], sqo[p_][:, k, :], k == 0, k == KC - 1, ["onesb", "sqo%d" % p_], ["ps4"])
            rstd_from(ps[4][:], "ps4", rs2[:], "rs2", D)
            for k in range(KC):
                dve(lambda e, k=k: e.tensor_tensor(out=tmp[:, k % 2, :], in0=xt[p_][:, k, :], in1=rs2[:], op=ALU.mult),
                    ["xt3%d" % p_, "rs2"], ["tmp3%d" % (k % 2)])
                if is_moe:
                    act(h2f[p_][:, k, :], tmp[:, k % 2, :], AF.Identity, ["tmp3%d" % (k % 2), "gsf%d" % l, "mod%d" % l],
                        ["h2f%d_%d" % (p_, k)], bias=modsb[l][:, 24 + k:25 + k], scale=gsf[l][:, k:k + 1])
                    P.add("pool", lambda e, k=k: e.tensor_copy(out=h2[p_][:, k, :], in_=h2f[p_][:, k, :]),
                          reads=["h2f%d_%d" % (p_, k)], writes=["h2_%d" % p_])
                else:
                    act(h2[p_][:, k, :], tmp[:, k % 2, :], AF.Identity, ["tmp3%d" % (k % 2), "gsf%d" % l, "mod%d" % l], ["h2_%d" % p_],
                        bias=modsb[l][:, 24 + k:25 + k], scale=gsf[l][:, k:k + 1])
            dma("sp", H2_d[:, :, cs].rearrange("k p t -> p k t"), h2[p_][:], "h2_%d" % p_, reads=["h2_%d" % p_], writes=["H2.%d" % c])
            if is_moe:
                for tt in range(4):
                    for k in range(KC):
                        mm(ps[5][:, tt * NE:(tt + 1) * NE], h2f[p_][:, k, tt * 128:(tt + 1) * 128], rw[:, k, :], k == 0,
                           k == KC - 1, ["h2f%d_%d" % (p_, k), "rw"], ["ps5"])
                dve(lambda e: e.tensor_copy(out=lg[:], in_=ps[5][:, 0:4 * NE].rearrange("p (t e) -> p t e", t=4)),
                    ["ps5"], ["lg"])
                dve(lambda e: e.tensor_reduce(out=m1t[:], in_=lg[:], axis=AX.X, op=ALU.max), ["lg"], ["m1t"])
                dve(lambda e: e.tensor_scalar(out=nm1[:], in0=m1t[:], scalar1=-1.0, scalar2=None, op0=ALU.mult),
                    ["m1t"], ["nm1"])
                for tt in range(4):
                    dve(lambda e, tt=tt: e.tensor_scalar(out=eq[:, tt, :], in0=lg[:, tt, :], scalar1=m1t[:, tt:tt + 1],
                                                         scalar2=None, op0=ALU.is_equal), ["lg", "m1t"], ["eq"])
                dve(lambda e: e.scalar_tensor_tensor(out=mk[:], in0=eq[:], scalar=-1e30, in1=lg[:], op0=ALU.mult,
                                                     op1=ALU.add), ["eq", "lg"], ["mk"])
                dve(lambda e: e.tensor_reduce(out=m2t[:], in_=mk[:], axis=AX.X, op=ALU.max), ["mk"], ["m2t"])
                for tt in range(4):
                    dve(lambda e, tt=tt: e.tensor_scalar(out=selt[:, tt, :], in0=lg[:, tt, :], scalar1=m2t[:, tt:tt + 1],
                                                         scalar2=None, op0=ALU.is_ge), ["lg", "m2t"], ["selt"])
                    act(ex[:, tt, :], lg[:, tt, :], AF.Exp, ["lg", "nm1"], ["ex"], bias=nm1[:, tt:tt + 1], scale=1.0)
                    act(den[:, tt:tt + 1], m2t[:, tt:tt + 1], AF.Exp, ["m2t", "nm1"], ["den"], bias=nm1[:, tt:tt + 1],
                        scale=1.0)
                dve(lambda e: e.tensor_scalar(out=den[:], in0=den[:], scalar1=1.0, scalar2=None, op0=ALU.add),
                    ["den"], ["den"])
                dve(lambda e: e.reciprocal(out=den[:], in_=den[:]), ["den"], ["den"])
                for tt in range(4):
                    dve(lambda e, tt=tt: e.scalar_tensor_tensor(out=gt[:, tt, 0:NE], in0=ex[:, tt, :],
                                                                scalar=den[:, tt:tt + 1], in1=selt[:, tt, :],
                                                                op0=ALU.mult, op1=ALU.mult),
                        ["ex", "den", "selt"], ["gt"])
                for tt in range(4):
                    mm(ps[6][:, tt * 128:(tt + 1) * 128], gt[:, tt, :], identf[:], True, True, ["gt", "identf"], ["ps6"])
                dve(lambda e: e.tensor_copy(out=gT[:], in_=ps[6][:]), ["ps6"], ["gT"])
                for ex_ in range(NE):
                    pb = 6 + (ex_ % 2)
                    mm(ps[pb][:], sele[:, ex_, :], gT[:], True, True, ["sele", "gT"], ["ps%d" % pb])
                    act(gb[:, ex_, :], ps[pb][:], AF.Copy, ["ps%d" % pb], ["gb"])
                dma("sp", GB_d[:, :, cs].rearrange("e p t -> p e t"), gb[:], "gb", reads=["gb"], writes=["GB.%d" % c])
        if stop == "p3":
            raise _Stop()
        P.barrier()
        A.release(m3)

        m4 = A.mark()
        TB = min(T, 2048)
        NSB = TB // 512
        h2b = A.alloc("h2b", [128, KC, TB], BF16)
        yacc = A.alloc("yacc", [128, KC, TB], F32)
        wg = [A.alloc("wg%d" % i, [128, KC, 512], BF16) for i in range(2)]
        wu = [A.alloc("wu%d" % i, [128, KC, 512], BF16) for i in range(2)]
        wd = [A.alloc("wd%d" % i, [128, 4, D], BF16) for i in range(2)]
        actb = [A.alloc("actb%d" % i, [128, 4, 512], BF16) for i in range(2)]
        sg = [A.alloc("sg%d" % i, [128, 512], F32) for i in range(2)]
        ta = [A.alloc("ta%d" % i, [128, 512], F32) for i in range(2)] if is_moe else None
        gbe = [A.alloc("gbe%d" % i, [128, TB], F32) for i in range(2)] if is_moe else None
        xt = A.alloc("xt4", [128, KC, 512], F32)
        nexp = NE if is_moe else 1
        wcount = 0
        gcount = 0
        for blk in range(T // TB):
            bs = slice(blk * TB, (blk + 1) * TB)
            chunks = range(blk * NSB, (blk + 1) * NSB)
            dma("sp", h2b[:], H2_d[:, :, bs].rearrange("k p t -> p k t"), "h2b",
                reads=["H2.%d" % c for c in chunks], writes=["h2b"])
            for ei in range(nexp):
                if is_moe:
                    mi = moe_layers.index(l)
                    Wg, Wu, Wd = mg_d[mi, ei], mu_d[mi, ei], md_d[mi, ei]
                    gbb = gcount % 2
                    gcount += 1
                    dma("sp", gbe[gbb][:], GB_d[ei, :, bs], "gbe%d" % gbb, reads=["GB.%d" % c for c in chunks],
                        writes=["gbe%d" % gbb])
                else:
                    di = [x for x in range(depth) if x not in moe_layers].index(l)
                    Wg, Wu, Wd = dg_d[di], du_d[di], dd_d[di]
                for fg in range(NFG):
                    wb = wcount % 2
                    wcount += 1
                    fs = slice(fg * 512, (fg + 1) * 512)
                    dma("pool", wg[wb][:], Wg[:, fs].rearrange("(k p) n -> p k n", p=128), "wg%d" % wb,
                        writes=["wg%d" % wb])
                    dma("pool", wu[wb][:], Wu[:, fs].rearrange("(k p) n -> p k n", p=128), "wu%d" % wb,
                        writes=["wu%d" % wb])
                    dma("pool", wd[wb][:], Wd[fs, :].rearrange("(j p) n -> p j n", p=128), "wd%d" % wb,
                        writes=["wd%d" % wb])
                    for sbi in range(NSB):
                        ss = slice(sbi * 512, (sbi + 1) * 512)
                        ab = (wcount * NSB + sbi) % 2
                        akey = "actb%d" % ab
                        for jj in range(4):
                            g_i = jj % 2
                            pg, pu = g_i, 2 + g_i
                            for k in range(KC):
                                mm(ps[pg][:], wg[wb][:, k, jj * 128:(jj + 1) * 128], h2b[:, k, ss], k == 0, k == KC - 1,
                                   ["wg%d" % wb, "h2b"], ["ps%d" % pg])
                            for k in range(KC):
                                mm(ps[pu][:], wu[wb][:, k, jj * 128:(jj + 1) * 128], h2b[:, k, ss], k == 0, k == KC - 1,
                                   ["wu%d" % wb, "h2b"], ["ps%d" % pu])
                            act(sg[g_i][:], ps[pg][:], AF.Silu, ["ps%d" % pg], ["sg%d" % g_i])
                            if is_moe:
                                dve(lambda e, g_i=g_i, pu=pu: e.tensor_tensor(out=ta[g_i][:], in0=sg[g_i][:],
                                                                              in1=ps[pu][:], op=ALU.mult),
                                    ["sg%d" % g_i, "ps%d" % pu], ["ta%d" % g_i])
                                dve(lambda e, g_i=g_i, ab=ab, jj=jj, gbb=gbb, ss=ss: e.tensor_tensor(
                                    out=actb[ab][:, jj, :], in0=ta[g_i][:], in1=gbe[gbb][:, ss], op=ALU.mult),
                                    ["ta%d" % g_i, "gbe%d" % gbb], [akey + ".%d" % jj])
                            else:
                                dve(lambda e, g_i=g_i, pu=pu, ab=ab, jj=jj: e.tensor_tensor(
                                    out=actb[ab][:, jj, :], in0=sg[g_i][:], in1=ps[pu][:], op=ALU.mult),
                                    ["sg%d" % g_i, "ps%d" % pu], [akey + ".%d" % jj])
                        first = (ei == 0 and fg == 0)
                        for i in range(KC):
                            py = 4 + (i % 2)
                            for jj in range(4):
                                mm(ps[py][:], wd[wb][:, jj, i * 128:(i + 1) * 128], actb[ab][:, jj, :], jj == 0, jj == 3,
                                   ["wd%d" % wb, akey + ".%d" % jj], ["ps%d" % py])
                            ykey = "yacc.%d.%d" % (i, sbi)
                            if first:
                                act(yacc[:, i, ss], ps[py][:], AF.Copy, ["ps%d" % py], [ykey])
                            else:
                                dve(lambda e, i=i, py=py, ss=ss: e.tensor_tensor(out=yacc[:, i, ss], in0=ps[py][:],
                                                                                 in1=yacc[:, i, ss], op=ALU.add),
                                    ["ps%d" % py, ykey], [ykey])
            for sbi in range(NSB):
                c = blk * NSB + sbi
                cs = slice(c * 512, (c + 1) * 512)
                ss = slice(sbi * 512, (sbi + 1) * 512)
                dma("sp", xt[:], xT_d[:, :, cs].rearrange("k p t -> p k t"), "xt4", reads=["xT.%d" % c], writes=["xt4"])
                for i in range(KC):
                    dve(lambda e, i=i, ss=ss: e.scalar_tensor_tensor(out=xt[:, i, :], in0=yacc[:, i, ss],
                                                                     scalar=modsb[l][:, 40 + i:41 + i], in1=xt[:, i, :],
                                                                     op0=ALU.mult, op1=ALU.add),
                        ["yacc.%d.%d" % (i, sbi), "mod%d" % l, "xt4"], ["xt4"])
                dma("sp", xT_d[:, :, cs].rearrange("k p t -> p k t"), xt[:], "xt4", reads=["xt4"], writes=["xT.%d" % c])
        if stop == "p4":
            raise _Stop()
        P.barrier()
        A.release(m4)

    xt = [A.alloc("xt5%d" % i_, [128, KC, 512], F32) for i_ in range(2)]
    sq = A.alloc("sq5", [128, KC, 512], BF16)
    rs = A.alloc("rs5", [128, 512], F32)
    yT = A.alloc("yT", [128, KC, 512], F32)
    yo = [A.alloc("yo%d" % i, [128, D], F32) for i in range(2)]
    ocount = 0
    for c in range(NCH):
        cs = slice(c * 512, (c + 1) * 512)
        def p5_load(cc):
            dma("sp", xt[cc % 2][:], xT_d[:, :, cc * 512:(cc + 1) * 512].rearrange("k p t -> p k t"), "xt5%d" % (cc % 2),
                reads=["xT.%d" % cc], writes=["xt5%d" % (cc % 2)])
        if c == 0:
            p5_load(0)
        if c + 1 < NCH:
            p5_load(c + 1)
        p_ = c % 2
        act(sq[:], xt[p_][:], AF.Square, ["xt5%d" % p_], ["sq5"])
        for k in range(KC):
            mm(ps[0][:], onesb[:], sq[:, k, :], k == 0, k == KC - 1, ["onesb", "sq5"], ["ps0"])
        rstd_from(ps[0][:], "ps0", rs[:], "rs5", D)
        for k in range(KC):
            dve(lambda e, k=k: e.scalar_tensor_tensor(out=yT[:, k, :], in0=xt[p_][:, k, :], scalar=colsb[:, 150 + k:151 + k],
                                                      in1=rs[:], op0=ALU.mult, op1=ALU.mult),
                ["xt5%d" % p_, "rs5", "cols"], ["yT"])
        for tt in range(4):
            ob = ocount % 2
            ocount += 1
            for half in range(2):
                pb = 2 + 2 * (tt % 2) + half
                for kq in range(4):
                    k = half * 4 + kq
                    mm(ps[pb][:, kq * 128:(kq + 1) * 128], yT[:, k, tt * 128:(tt + 1) * 128], identf[:], True, True,
                       ["yT", "identf"], ["ps%d" % pb])
                if half == 0:
                    act(yo[ob][:, 0:512], ps[pb][:], AF.Copy, ["ps%d" % pb], ["yo%d" % ob])
                else:
                    dve(lambda e, ob=ob, pb=pb: e.tensor_copy(out=yo[ob][:, 512:1024], in_=ps[pb][:]), ["ps%d" % pb],
                        ["yo%d" % ob])
            r0 = c * 512 + tt * 128
            dma("sp", out_d[r0:r0 + 128, :], yo[ob][:], "yo%d" % ob, reads=["yo%d" % ob], writes=["out.%d" % (r0 // 128)])
    P.finish()
    P.emit()
    es.close()
    return nc, es


def _col(v):
    v = np.asarray(v, dtype=np.float32)
    return np.ascontiguousarray(v.reshape(-1, 128).T)


def _const_tables():
    cf = np.zeros((128, CONSTF), np.float32)
    cf[:, 0:128] = np.eye(128, dtype=np.float32)
    cf[:, 128:256] = 1.0
    p = np.arange(128)[:, None]
    q = np.arange(512)[None, :]
    for j in range(4):
        cf[:, 256 + j * 512:256 + (j + 1) * 512] = np.where(j * 128 + p > q, -30000.0, 0.0)
    cf[64, 2304:2400] = 1.0
    for e in range(NE):
        cf[e, 2400 + e * 128:2400 + (e + 1) * 128] = 1.0
    half = 16
    inv_freq = (np.float32(10000.0) ** (-np.arange(half, dtype=np.float32) / np.float32(half))).astype(np.float32)
    rope = np.zeros((128, 2), np.float32)
    rope[:, 0] = inv_freq[np.arange(128) % 16]
    rope[:, 1] = np.where((np.arange(128) % 32) < 16, -1.0, 1.0)
    return cf, rope


def prepare_inputs(depth, moe_layers, T, x, c, positions, ada_w, ada_b, attn_norm_g, w_in, q_norm_g, w_uq, kv_norm_g,
                   w_ukv, fox_forget_b, mla_out_g, fox_out_g, w_o, ffn_norm_g, dense_w_gate, dense_w_up, dense_w_down,
                   router_w, moe_w_gate, moe_w_up, moe_w_down, final_norm_g, ncores):
    f32 = lambda a: np.ascontiguousarray(np.asarray(a, dtype=np.float32))
    cf, rope = _const_tables()
    w_in = f32(w_in)[:depth]
    w_uq, w_ukv, ada_w, w_o = f32(w_uq)[:depth], f32(w_ukv)[:depth], f32(ada_w)[:depth], f32(w_o)[:depth]
    cq_, ckv_, kr_, fq_, fk_, fv_, fl_ = (w_in[:, :, 0:256], w_in[:, :, 256:384], w_in[:, :, 384:416],
                                          w_in[:, :, 416:928], w_in[:, :, 928:1440], w_in[:, :, 1440:1952],
                                          w_in[:, :, 1952:1960])
    krb = np.concatenate([kr_[:, :, 16:32], kr_[:, :, 0:16]], axis=-1)
    w_in_p = np.ascontiguousarray(np.concatenate([cq_, ckv_, kr_, ckv_[:, :, 64:128], krb, fl_, fq_, fk_, fv_], axis=-1))
    assert w_in_p.shape[-1] == WIN_COLS
    w_uq = f32(w_uq).reshape(depth, 256, NH, 96)
    wqb = np.concatenate([w_uq[..., 0:64], w_uq[..., 80:96], w_uq[..., 64:80]], axis=-1)
    w_uqa = np.ascontiguousarray(w_uq.reshape(depth, 256, 768))
    w_uqb = np.ascontiguousarray(wqb.reshape(depth, 256, 768))
    wkv = f32(w_ukv).reshape(depth, 128, NH, 128)
    w_ukv_p = np.ascontiguousarray(np.concatenate([wkv[..., 0:64].reshape(depth, 128, 512),
                                                   wkv[..., 64:128].reshape(depth, 128, 512)], axis=-1))
    shared = {
        "constf": cf, "ada_w": f32(ada_w), "w_in": w_in_p, "w_uqa": w_uqa, "w_uqb": w_uqb, "w_ukv": w_ukv_p,
        "w_o": f32(w_o), "dense_w_gate": f32(dense_w_gate), "dense_w_up": f32(dense_w_up),
        "dense_w_down": f32(dense_w_down), "router_w": f32(router_w), "moe_w_gate": f32(moe_w_gate),
        "moe_w_up": f32(moe_w_up), "moe_w_down": f32(moe_w_down),
    }
    in_maps = []
    for b in range(ncores):
        cols = np.zeros((128, NCOL), np.float32)
        for l in range(depth):
            o = l * LCOLS
            cols[:, o:o + 8] = _col(attn_norm_g[l])
            cols[:, o + 8:o + 56] = _col(ada_b[l])
            cols[:, o + 56:o + 58] = _col(q_norm_g[l])
            cols[:, o + 58:o + 59] = _col(kv_norm_g[l])
            cols[:, o + 59:o + 63] = _col(mla_out_g[l])
            cols[:, o + 63:o + 67] = _col(fox_out_g[l])
            cols[:, o + 67:o + 75] = _col(ffn_norm_g[l])
            cols[0:8, 168 + l] = np.asarray(fox_forget_b[l], np.float32)
        cols[:, 150:158] = _col(final_norm_g)
        cols[:, 158:166] = _col(np.asarray(c)[b])
        cols[:, 166:168] = rope
        m = dict(shared)
        m["x"] = f32(np.asarray(x)[b, :T])
        m["posb"] = np.ascontiguousarray(np.broadcast_to(np.asarray(positions)[b, :T].astype(np.int32), (128, T)))
        m["cols"] = cols
        in_maps.append(m)
    return in_maps


_CACHE = {}


def run_config(T, depth, moe_layers, ncores, inputs, debug=False, stop=None):
    key = (T, depth, tuple(moe_layers), debug, stop)
    if key not in _CACHE:
        _CACHE[key] = build_program(T, depth, list(moe_layers), debug=debug, stop=stop)
    nc, _es = _CACHE[key]
    in_maps = prepare_inputs(depth, list(moe_layers), T, ncores=ncores, **inputs)
    res = run_bass_kernel_spmd(nc, in_maps, core_ids=list(range(ncores)))
    out = np.stack([np.asarray(r["out"]) for r in res.results], axis=0)
    if debug:
        return out.astype(np.float32), res.results
    return out.astype(np.float32)


def kernel(**inputs):
    return run_config(4096, 2, [1], 8, inputs)
```

```python
import math
from contextlib import ExitStack

import numpy as np
import concourse.bass as bass
import concourse.mybir as mybir
from concourse.bass_utils import run_bass_kernel_spmd

F32 = mybir.dt.float32
BF16 = mybir.dt.bfloat16
I32 = mybir.dt.int32
AF = mybir.ActivationFunctionType
ALU = mybir.AluOpType
AX = mybir.AxisListType

D = 1024
KC = 8
NH = 8
DFF = 3584
NFG = 7
NE = 8
WIN_COLS = 2056
EPS = 1e-6
NCOL = 170
LCOLS = 75
CONSTF = 128 + 128 + 2048 + 96 + 1024
MLA_SCALE = 96 ** -0.5
TWO_PI = 2.0 * math.pi

DSIZE = {F32: 4, BF16: 2, I32: 4}


class Op:
    __slots__ = ("eng", "fn", "deps", "sem", "ticket", "signal", "is_dma", "idx")


class _Rec:
    def __init__(self):
        self.calls = []

    def __getattr__(self, name):
        def f(*a, **k):
            self.calls.append((name, a, k))
            return None
        return f


class Prog:
    ENGS = ("pe", "act", "dve", "pool", "sp")

    def __init__(self, nc, es):
        self.nc = nc
        self.es = es
        self.ops = {e: [] for e in self.ENGS}
        self.semh = {}
        self.dmacount = {}
        self.lastw = {}
        self.readers = {}
        self.lastdma = {}
        self.pending = {e: [] for e in self.ENGS}
        self.dma_since_bar = []
        self.final_deps = []
        self.n = 0

    def sem(self, name):
        if name not in self.semh:
            self.semh[name] = self.es.enter_context(self.nc.semaphore("s%d" % len(self.semh)))
        return self.semh[name]

    def add(self, eng, fn, reads=(), writes=(), dma_key=None):
        op = Op()
        op.eng = eng
        rec = _Rec()
        fn(rec)
        assert len(rec.calls) == 1
        cname, cargs, ckw = rec.calls[0]
        op.fn = lambda e, _n=cname, _a=cargs, _k=ckw: getattr(e, _n)(*_a, **_k)
        op.is_dma = dma_key is not None
        op.signal = op.is_dma
        op.ticket = None
        op.idx = self.n
        self.n += 1
        deps = {}
        for r in reads:
            w = self.lastw.get(r)
            if w is not None:
                deps[id(w)] = w
        for w_ in writes:
            lw = self.lastw.get(w_)
            if lw is not None:
                deps[id(lw)] = lw
            for rd in self.readers.get(w_, {}).values():
                deps[id(rd)] = rd
        if op.is_dma:
            ld = self.lastdma.get(dma_key)
            if ld is not None:
                deps[id(ld)] = ld
        for p in self.pending[eng]:
            deps[id(p)] = p
        self.pending[eng] = []
        deps.pop(id(op), None)
        if op.is_dma:
            op.sem = "d:" + dma_key
            c = self.dmacount.get(op.sem, 0) + 16
            self.dmacount[op.sem] = c
            op.ticket = c
            self.lastdma[dma_key] = op
            self.dma_since_bar.append(op)
        else:
            op.sem = "e:" + eng
        final = []
        for d in deps.values():
            if (not d.is_dma) and d.eng == "pe" and eng == "pe" and not op.is_dma:
                continue
            d.signal = True
            final.append(d)
        op.deps = final
        rk = op.sem
        for r in reads:
            self.readers.setdefault(r, {})[rk] = op
        for w_ in writes:
            self.lastw[w_] = op
            self.readers[w_] = {}
        self.ops[eng].append(op)
        return op

    def barrier(self):
        deps = []
        for e in self.ENGS:
            if self.ops[e]:
                deps.append(self.ops[e][-1])
        deps.extend(self.dma_since_bar)
        self.dma_since_bar = []
        for d in deps:
            d.signal = True
        for e in self.ENGS:
            self.pending[e] = list(deps)

    def finish(self):
        self.barrier()
        self.final_deps = list(self.pending["sp"])

    def emit(self):
        nc = self.nc
        for e in self.ENGS:
            c = 0
            for op in self.ops[e]:
                if not op.is_dma and op.signal:
                    c += 1
                    op.ticket = c
        handles = {"pe": "tensor", "act": "scalar", "dve": "vector", "pool": "gpsimd", "sp": "sync"}
        import os
        for i_ in range(int(os.environ.get("SEM_SHIFT", "0"))):
            self.sem("dummy%d" % i_)
        names = []
        for e in self.ENGS:
            for op in self.ops[e]:
                if op.signal and op.sem not in names:
                    names.append(op.sem)
        hs = [self.sem(nm) for nm in names]
        with nc.Block() as b0:
            b0.sync(lambda eng: [eng.sem_clear(h) for h in hs])

        def run(ename, eng):
            waited = {}

            def do_waits(deps):
                need = {}
                for d in deps:
                    if d.ticket is None:
                        continue
                    if need.get(d.sem, 0) < d.ticket:
                        need[d.sem] = d.ticket
                for s, v in need.items():
                    if waited.get(s, 0) < v:
                        eng.wait_ge(self.sem(s), v)
                        waited[s] = v

            for op in self.ops[ename]:
                do_waits(op.deps)
                ins = op.fn(eng)
                if op.signal:
                    ins.then_inc(self.sem(op.sem), 16 if op.is_dma else 1)
            if ename == "sp":
                do_waits(self.final_deps)

        with nc.Block() as block:
            for ename in self.ENGS:
                getattr(block, handles[ename])(lambda eng, _n=ename: run(_n, eng))


class Arena:
    def __init__(self, nc, limit):
        self.nc = nc
        self.off = 16512
        self.limit = 16512 + limit
        self.cnt = 0

    def alloc(self, name, shape, dtype):
        n = 1
        for s in shape[1:]:
            n *= s
        nbytes = n * DSIZE[dtype]
        nbytes = (nbytes + 63) // 64 * 64
        assert self.off + nbytes <= self.limit, (name, self.off, nbytes, self.limit)
        self.cnt += 1
        t = self.nc.alloc_sbuf_tensor_at("%s_%d" % (name, self.cnt), list(shape), dtype, offset=self.off)
        self.off += nbytes
        return t

    def mark(self):
        return self.off

    def release(self, m):
        self.off = m


class _Stop(Exception):
    pass


def build_program(T, depth, moe_layers, debug=False, stop=None):
    nc_holder = []
    try:
        return _build_program(T, depth, moe_layers, debug, stop, nc_holder)
    except _Stop:
        nc, es, P = nc_holder
        P.finish()
        P.emit()
        es.close()
        return nc, es


def _build_program(T, depth, moe_layers, debug, stop, nc_holder):
    NCH = T // 512
    NT = T // 128
    nc = bass.Bass("TRN2", target_bir_lowering=False)
    es = ExitStack()
    P = Prog(nc, es)
    nc_holder.extend([nc, es, P])

    def din(name, shape, dt=F32):
        return nc.dram_tensor(name, list(shape), dt, kind="ExternalInput").ap()

    def dscr(name, shape, dt):
        return nc.dram_tensor(name, list(shape), dt, kind=("ExternalOutput" if debug else "Internal")).ap()

    x_d = din("x", [T, D])
    posb_d = din("posb", [128, T], I32)
    cols_d = din("cols", [128, NCOL])
    constf_d = din("constf", [128, CONSTF])
    adaw_d = din("ada_w", [depth, D, 6 * D])
    win_d = din("w_in", [depth, D, WIN_COLS])
    wqa_d = din("w_uqa", [depth, 256, 768])
    wqb_d = din("w_uqb", [depth, 256, 768])
    wkv_d = din("w_ukv", [depth, 128, 1024])
    wo_d = din("w_o", [depth, D, D])
    n_moe = max(1, len(moe_layers))
    n_dense = max(1, depth - len(moe_layers))
    dg_d = din("dense_w_gate", [n_dense, D, DFF])
    du_d = din("dense_w_up", [n_dense, D, DFF])
    dd_d = din("dense_w_down", [n_dense, DFF, D])
    rw_d = din("router_w", [n_moe, D, NE])
    mg_d = din("moe_w_gate", [n_moe, NE, D, DFF])
    mu_d = din("moe_w_up", [n_moe, NE, D, DFF])
    md_d = din("moe_w_down", [n_moe, NE, DFF, D])
    out_d = nc.dram_tensor("out", [T, D], F32, kind="ExternalOutput").ap()

    xT_d = dscr("xT", [KC, 128, T], F32)
    QM_d = dscr("QM", [NH, 96, T], BF16)
    KM_d = dscr("KM", [NH, 96, T], BF16)
    VM_d = dscr("VM", [NH, 128, NT, 65], BF16)
    QF_d = dscr("QF", [4, 128, T], BF16)
    KF_d = dscr("KF", [4, 128, T], BF16)
    QA_d = dscr("QA", [NH, 6, T], BF16)
    KA_d = dscr("KA", [NH, 6, T], BF16)
    VF_d = dscr("VF", [NH, 128, NT, 65], BF16)
    OT_d = dscr("OT", [KC, 128, T], BF16)
    H2_d = dscr("H2", [KC, 128, T], BF16)
    GB_d = dscr("GB", [NE, 128, T], F32)

    A = Arena(nc, 206 * 1024)
    ps = [nc.alloc_psum_tensor("ps%d" % i, [128, 512], F32) for i in range(8)]

    colsb = A.alloc("cols", [128, NCOL], F32)
    identf = A.alloc("identf", [128, 128], F32)
    sell = A.alloc("sell", [128, 96], F32)
    sele = A.alloc("sele", [128, NE, 128], F32)
    identb = A.alloc("identb", [128, 128], BF16)
    onesb = A.alloc("onesb", [128, 128], BF16)
    maskb = A.alloc("maskb", [128, 4, 512], BF16)
    modsb = [A.alloc("mod%d" % l, [128, 48], F32) for l in range(depth)]
    gsa = [A.alloc("gsa%d" % l, [128, 8], F32) for l in range(depth)]
    gsf = [A.alloc("gsf%d" % l, [128, 8], F32) for l in range(depth)]
    cact = A.alloc("cact", [128, 8], F32)
    negfb = A.alloc("negfb", [128, 2], F32)
    sgn2pi = A.alloc("sgn2pi", [128, 1], F32)

    def dma(q, out, in_, key, reads=(), writes=()):
        return P.add(q, lambda e, o=out, i=in_: e.dma_start(out=o, in_=i), reads=reads, writes=writes, dma_key=key)

    def mm(out, lhsT, rhs, start, stop, reads, writes):
        return P.add("pe", lambda e, o=out, l=lhsT, r=rhs, s=start, t=stop: e.matmul(o, l, r, start=s, stop=t),
                     reads=reads, writes=writes)

    def act(out, in_, func, reads, writes, bias=None, scale=None):
        kw = {}
        if bias is not None:
            kw["bias"] = bias
        if scale is not None:
            kw["scale"] = scale
        return P.add("act", lambda e, o=out, i=in_, f=func, k=kw: e.activation(out=o, in_=i, func=f, **k),
                     reads=reads, writes=writes)

    def dve(fn, reads, writes, eng="dve"):
        return P.add(eng, fn, reads=reads, writes=writes)

    def dbg(name, ap, shape, dt, keys):
        if not debug:
            return
        d_ = nc.dram_tensor("dbg_" + name, list(shape), dt, kind="ExternalOutput").ap()
        dma("sp", d_, ap, "dbg_" + name, reads=keys, writes=["dbg_" + name])

    dma("sp", colsb[:], cols_d, "cols", writes=["cols"])
    dma("sp", identf[:], constf_d[:, 0:128], "identf", writes=["identf"])
    dma("sp", sell[:], constf_d[:, 2304:2400], "sell", writes=["sell"])
    dma("sp", sele[:], constf_d[:, 2400:3424].rearrange("p (e m) -> p e m", e=NE), "sele", writes=["sele"])
    dma("pool", identb[:], constf_d[:, 0:128], "identb", writes=["identb"])
    dma("pool", onesb[:], constf_d[:, 128:256], "onesb", writes=["onesb"])
    dma("pool", maskb[:], constf_d[:, 256:2304].rearrange("p (j q) -> p j q", j=4), "maskb", writes=["maskb"])

    act(cact[:], colsb[:, 158:166], AF.Silu, ["cols"], ["cact"])
    dve(lambda e: e.tensor_scalar(out=negfb[:], in0=colsb[:, 168:170], scalar1=-1.0, scalar2=None, op0=ALU.mult),
        ["cols"], ["negfb"])
    dve(lambda e: e.tensor_scalar(out=sgn2pi[:], in0=colsb[:, 167:168], scalar1=TWO_PI * (1.0 - 2e-6), scalar2=None,
                                  op0=ALU.mult), ["cols"], ["sgn2pi"])

    m0 = A.mark()
    awb = [A.alloc("aw%d" % i, [128, KC, 512], F32) for i in range(2)]
    for l in range(depth):
        for jg in range(12):
            b = (l * 12 + jg) % 2
            dma("sp", awb[b][:], adaw_d[l, :, jg * 512:(jg + 1) * 512].rearrange("(k p) n -> p k n", p=128),
                "aw%d" % b, writes=["aw%d" % b])
            for jj in range(4):
                j = jg * 4 + jj
                for k in range(KC):
                    mm(ps[0][:, j:j + 1], awb[b][:, k, jj * 128:(jj + 1) * 128], cact[:, k:k + 1],
                       k == 0, k == KC - 1, ["aw%d" % b, "cact"], ["ps0"])
        c0 = l * LCOLS
        dve(lambda e, l=l, c0=c0: e.tensor_tensor(out=modsb[l][:], in0=ps[0][:, 0:48], in1=colsb[:, c0 + 8:c0 + 56],
                                                  op=ALU.add), ["ps0", "cols"], ["mod%d" % l])
        dve(lambda e, l=l, c0=c0: e.scalar_tensor_tensor(out=gsa[l][:], in0=modsb[l][:, 8:16], scalar=1.0,
                                                         in1=colsb[:, c0:c0 + 8], op0=ALU.add, op1=ALU.mult),
            ["mod%d" % l, "cols"], ["gsa%d" % l])
        dve(lambda e, l=l, c0=c0: e.scalar_tensor_tensor(out=gsf[l][:], in0=modsb[l][:, 32:40], scalar=1.0,
                                                         in1=colsb[:, c0 + 67:c0 + 75], op0=ALU.add, op1=ALU.mult),
            ["mod%d" % l, "cols"], ["gsf%d" % l])
    dbg("mod0", modsb[0][:], [128, 48], F32, ["mod0"])
    dbg("gsa0", gsa[0][:], [128, 8], F32, ["gsa0"])
    dbg("cact", cact[:], [128, 8], F32, ["cact"])
    dbg("cols", colsb[:], [128, NCOL], F32, ["cols"])
    dbg("onesb", onesb[:], [128, 128], BF16, ["onesb"])
    P.barrier()
    A.release(m0)

    def rstd_from(psb, pskey, rs, rskey, n):
        act(rs, psb, AF.Ln, [pskey], [rskey], bias=EPS, scale=1.0 / n)
        act(rs, rs, AF.Exp, [rskey], [rskey], scale=-0.5)

    for l in range(depth):
        c0 = l * LCOLS
        is_moe = l in moe_layers
        m1 = A.mark()
        wi = A.alloc("wi", [128, KC, WIN_COLS], BF16)
        wqa = A.alloc("wqa", [128, 2, 768], BF16)
        wqb = A.alloc("wqb", [128, 2, 768], BF16)
        wkv = A.alloc("wkv", [128, 1024], BF16)
        for k in range(KC):
            dma("pool", wi[:, k, :], win_d[l, k * 128:(k + 1) * 128, :], "wi%d" % k, writes=["wi%d" % k])
        WI = ["wi%d" % k for k in range(KC)]
        dma("pool", wqa[:], wqa_d[l].rearrange("(j p) n -> p j n", p=128), "wqa", writes=["wqa"])
        dma("pool", wqb[:], wqb_d[l].rearrange("(j p) n -> p j n", p=128), "wqb", writes=["wqb"])
        dma("pool", wkv[:], wkv_d[l], "wkv", writes=["wkv"])
        xin = A.alloc("xin", [128, 4, D], F32) if l == 0 else None
        xt = A.alloc("xt", [128, KC, 512], F32)
        sq = A.alloc("sq", [128, KC, 512], BF16)
        tmp = A.alloc("tmp", [128, 2, 512], F32)
        hT = A.alloc("hT", [128, KC, 512], BF16)
        rs = A.alloc("rs", [128, 512], F32)
        cq = A.alloc("cq", [128, 2, 512], F32)
        sqq = A.alloc("sqq", [128, 2, 512], BF16)
        rsq = A.alloc("rsq", [128, 512], F32)
        qn = A.alloc("qn", [128, 2, 512], BF16)
        ckv = A.alloc("ckv", [128, 512], F32)
        sqk = A.alloc("sqk", [128, 512], BF16)
        rsk = A.alloc("rsk", [128, 512], F32)
        kvn = A.alloc("kvn", [128, 512], BF16)
        qt = A.alloc("qt", [96, NH, 512], BF16)
        kt_ = A.alloc("kt", [96, NH, 512], BF16)
        krope = A.alloc("krope", [96, 512], BF16)
        r1 = A.alloc("r1", [96, 512], F32)
        r2 = A.alloc("r2", [96, 512], F32)
        vm = A.alloc("vm", [128, NH, 4, 65], BF16)
        vf = A.alloc("vf", [128, NH, 4, 65], BF16)
        fq = A.alloc("fq", [128, 4, 512], BF16)
        fk = A.alloc("fk", [128, 4, 512], BF16)
        posi = A.alloc("posi", [128, 512], I32)
        rr = A.alloc("rr", [128, 512], F32)
        ri = A.alloc("ri", [128, 512], I32)
        rf = A.alloc("rf", [128, 512], F32)
        ff = A.alloc("ff", [128, 512], F32)
        gg = A.alloc("gg", [128, 512], F32)
        cosb = A.alloc("cosb", [128, 512], F32)
        sinb = A.alloc("sinb", [128, 512], F32)
        ez = A.alloc("ez", [8, 512], F32)
        lp = A.alloc("lp", [8, 512], F32)
        ones8 = A.alloc("ones8", [8, 512], F32)
        Fc = [A.alloc("Fc%d" % i, [8, 512], F32) for i in range(2)]
        fr = A.alloc("fr", [8, 512], F32)
        fb = A.alloc("fb", [8, 512], BF16)
        augq = A.alloc("augq", [8, 6, 512], BF16)
        augk = A.alloc("augk", [8, 6, 512], BF16)

        dve(lambda e: e.memset(vm[:], 1.0), [], ["vm"])
        dve(lambda e: e.memset(vf[:], 1.0), [], ["vf"])
        dve(lambda e: e.memset(ones8[:], 1.0), [], ["ones8"])
        dve(lambda e: e.memset(augq[:], 1.0), [], ["augq"])
        dve(lambda e: e.memset(augk[:], 1.0), [], ["augk"])

        def p1_loads(cc):
            cs_ = slice(cc * 512, (cc + 1) * 512)
            if l == 0:
                dma("sp", xin[:], x_d[cs_, :].rearrange("(t p) d -> p t d", p=128), "xin", writes=["xin"])
            else:
                dma("sp", xt[:], xT_d[:, :, cs_].rearrange("k p t -> p k t"), "xt", reads=["xT.%d" % cc],
                    writes=["xt"])
            dma("sp", posi[:], posb_d[:, cs_], "posi", writes=["posi"])
        def p1_front(c):
            cs = slice(c * 512, (c + 1) * 512)
            if c == 0:
                p1_loads(0)
            if l == 0:
                for k in range(KC):
                    pb = k % 2
                    for tt in range(4):
                        mm(ps[pb][:, tt * 128:(tt + 1) * 128], xin[:, tt, k * 128:(k + 1) * 128], identf[:], True, True,
                           ["xin", "identf"], ["ps%d" % pb])
                    act(xt[:, k, :], ps[pb][:], AF.Copy, ["ps%d" % pb], ["xt"])
                dma("sp", xT_d[:, :, cs].rearrange("k p t -> p k t"), xt[:], "xt", reads=["xt"], writes=["xT.%d" % c])
            act(sq[:], xt[:], AF.Square, ["xt"], ["sq"])
            for k in range(KC):
                mm(ps[2][:], onesb[:], sq[:, k, :], k == 0, k == KC - 1, ["onesb", "sq"], ["ps2"])
            rstd_from(ps[2][:], "ps2", rs[:], "rs", D)
            for k in range(KC):
                dve(lambda e, k=k: e.tensor_tensor(out=tmp[:, k % 2, :], in0=xt[:, k, :], in1=rs[:], op=ALU.mult),
                    ["xt", "rs"], ["tmp%d" % (k % 2)])
                act(hT[:, k, :], tmp[:, k % 2, :], AF.Identity, ["tmp%d" % (k % 2), "gsa%d" % l, "mod%d" % l], ["hT%d" % k],
                    bias=modsb[l][:, k:k + 1], scale=gsa[l][:, k:k + 1])
            HT = ["hT%d" % k for k in range(KC)]
            if c == 0 and l == 0:
                dbg("xt", xt[:], [128, KC, 512], F32, ["xt"])
                if stop == "xt":
                    raise _Stop()
                dbg("sq", sq[:], [128, KC, 512], BF16, ["sq"])
                dbg("rs", rs[:], [128, 512], F32, ["rs"])
                dbg("hT", hT[:], [128, KC, 512], BF16, HT)
                dbg("wi", wi[:], [128, KC, WIN_COLS], BF16, WI)
                if stop == "hT":
                    raise _Stop()

        def p1_mid(c):
            cs = slice(c * 512, (c + 1) * 512)
            HT = ["hT%d" % k for k in range(KC)]
            def proj(pb, col0, ncols, extra_reads=()):
                for k in range(KC):
                    mm(ps[pb][0:ncols, :], wi[:, k, col0:col0 + ncols], hT[:, k, :], k == 0, k == KC - 1,
                       [WI[k], HT[k]] + list(extra_reads), ["ps%d" % pb])

            dve(lambda e: e.tensor_copy(out=rr[:], in_=posi[:]), ["posi"], ["rr"])
            dve(lambda e: e.tensor_scalar(out=rr[:], in0=rr[:], scalar1=colsb[:, 166:167], scalar2=1.0 / TWO_PI,
                                          op0=ALU.mult, op1=ALU.mult), ["rr", "cols"], ["rr"])
            for which in range(2):
                dst = sinb if which == 0 else cosb
                dkey = "sinb" if which == 0 else "cosb"
                if which == 1:
                    dve(lambda e: e.tensor_scalar(out=rr[:], in0=rr[:], scalar1=0.25, scalar2=None, op0=ALU.add),
                        ["rr"], ["rr"])
                dve(lambda e: e.tensor_copy(out=ri[:], in_=rr[:]), ["rr"], ["ri"])
                dve(lambda e: e.tensor_copy(out=rf[:], in_=ri[:]), ["ri"], ["rf"])
                dve(lambda e: e.tensor_tensor(out=ff[:], in0=rr[:], in1=rf[:], op=ALU.subtract), ["rr", "rf"], ["ff"])
                dve(lambda e: e.tensor_scalar(out=gg[:], in0=ff[:], scalar1=0.5, scalar2=None, op0=ALU.is_gt),
                    ["ff"], ["gg"])
                dve(lambda e: e.tensor_tensor(out=ff[:], in0=ff[:], in1=gg[:], op=ALU.subtract), ["ff", "gg"], ["ff"])
                dve(lambda e: e.tensor_scalar(out=gg[:], in0=ff[:], scalar1=-0.5, scalar2=None, op0=ALU.is_lt),
                    ["ff"], ["gg"])
                dve(lambda e: e.tensor_tensor(out=ff[:], in0=ff[:], in1=gg[:], op=ALU.add), ["ff", "gg"], ["ff"])
                if which == 0:
                    act(dst[:], ff[:], AF.Sin, ["ff", "sgn2pi"], [dkey], scale=sgn2pi[:, 0:1])
                else:
                    act(dst[:], ff[:], AF.Sin, ["ff"], [dkey], scale=TWO_PI * (1.0 - 2e-6))

            if c + 1 < NCH:
                p1_loads(c + 1)

            def rope_rows(psa, psb, pka, pkb, dst, dkey):
                dve(lambda e: e.tensor_tensor(out=r1[64:96, :], in0=psa[64:96, :], in1=cosb[64:96, :], op=ALU.mult),
                    [pka, "cosb"], ["r1"])
                dve(lambda e: e.tensor_tensor(out=r2[64:96, :], in0=psb[64:96, :], in1=sinb[64:96, :], op=ALU.mult),
                    [pkb, "sinb"], ["r2"])
                dve(lambda e, dst=dst: e.tensor_tensor(out=dst, in0=r1[64:96, :], in1=r2[64:96, :], op=ALU.add),
                    ["r1", "r2"], [dkey])

            if stop == "rope":
                raise _Stop()
            for j in range(2):
                proj(3 + j, j * 128, 128)
                act(cq[:, j, :], ps[3 + j][:], AF.Copy, ["ps%d" % (3 + j)], ["cq%d" % j])
            if stop == "cq":
                raise _Stop()
            proj(5, 256, 128)
            act(ckv[:], ps[5][:], AF.Copy, ["ps5"], ["ckv"])
            if stop == "ckv":
                raise _Stop()
            proj(6, 320, 96)
            proj(7, 416, 96)
            rope_rows(ps[6], ps[7], "ps6", "ps7", krope[64:96, :], "krope")
            if stop == "krope":
                raise _Stop()
            proj(3, 512, 128)
            act(ez[:], ps[3][0:8, :], AF.Exp, ["ps3", "negfb"], ["ez"], bias=negfb[0:8, l:l + 1], scale=-1.0)
            act(lp[:], ez[:], AF.Ln, ["ez"], ["lp"], bias=1.0)
            cur, prev = Fc[c % 2], Fc[(c + 1) % 2]
            init = 0.0 if c == 0 else prev[:, 511:512]
            dve(lambda e, cur=cur, init=init: e.tensor_tensor_scan(out=cur[:], data0=ones8[:], data1=lp[:], initial=init,
                                                                   op0=ALU.mult, op1=ALU.subtract),
                ["ones8", "lp", "Fc%d" % ((c + 1) % 2)], ["Fc%d" % (c % 2)])
            fkey = "Fc%d" % (c % 2)
            dve(lambda e, cur=cur: e.tensor_copy(out=augq[:, 0, :], in_=cur[:]), [fkey], ["augq"])
            dve(lambda e, cur=cur: e.tensor_tensor(out=fr[:], in0=cur[:], in1=augq[:, 0, :], op=ALU.subtract),
                [fkey, "augq"], ["fr"])
            dve(lambda e: e.tensor_copy(out=augq[:, 1, :], in_=fr[:]), ["fr"], ["augq"])
            dve(lambda e: e.tensor_tensor(out=fr[:], in0=fr[:], in1=augq[:, 1, :], op=ALU.subtract), ["fr", "augq"], ["fr"])
            dve(lambda e: e.tensor_copy(out=augq[:, 2, :], in_=fr[:]), ["fr"], ["augq"])
            for i3 in range(3):
                dve(lambda e, i3=i3: e.tensor_scalar(out=augk[:, 3 + i3, :], in0=augq[:, i3, :], scalar1=-1.0,
                                                     scalar2=None, op0=ALU.mult), ["augq"], ["augk"])
            dma("sp", QA_d[:, :, cs], augq[:], "augq", reads=["augq"], writes=["QA.%d" % c])
            dma("sp", KA_d[:, :, cs], augk[:], "augk", reads=["augk"], writes=["KA.%d" % c])
            if stop == "gates":
                raise _Stop()
            for j in range(4):
                pb = 4 + (j % 2)
                proj(pb, 520 + j * 128, 128)
                act(fq[:, j, :], ps[pb][:], AF.Copy, ["ps%d" % pb], ["fq"], scale=0.125)
            dma("sp", QF_d[:, :, cs].rearrange("j p t -> p j t"), fq[:], "fq", reads=["fq"], writes=["QF.%d" % c])
            for j in range(4):
                pb = 6 + (j % 2)
                proj(pb, 1032 + j * 128, 128)
                dve(lambda e, j=j, pb=pb: e.tensor_copy(out=fk[:, j, :], in_=ps[pb][:]), ["ps%d" % pb], ["fk"])
            dma("sp", KF_d[:, :, cs].rearrange("j p t -> p j t"), fk[:], "fk", reads=["fk"], writes=["KF.%d" % c])
            if stop == "fqk":
                raise _Stop()
            for tt in range(4):
                pb = 3 + (tt % 2)
                for k in range(KC):
                    mm(ps[pb][:], hT[:, k, tt * 128:(tt + 1) * 128], wi[:, k, 1544:2056], k == 0, k == KC - 1,
                       [HT[k], WI[k]], ["ps%d" % pb])
                act(vf[:, :, tt, 0:64], ps[pb][:].rearrange("p (h d) -> p h d", h=NH), AF.Copy, ["ps%d" % pb], ["vf"])
            dma("sp", VF_d[:, :, c * 4:(c + 1) * 4, :].rearrange("h p t d -> p h t d"), vf[:], "vf", reads=["vf"],
                writes=["VF.%d" % c])
        def p1_tail(c):
            cs = slice(c * 512, (c + 1) * 512)
            def rope_rows(psa, psb, pka, pkb, dst, dkey):
                dve(lambda e: e.tensor_tensor(out=r1[64:96, :], in0=psa[64:96, :], in1=cosb[64:96, :], op=ALU.mult),
                    [pka, "cosb"], ["r1"])
                dve(lambda e: e.tensor_tensor(out=r2[64:96, :], in0=psb[64:96, :], in1=sinb[64:96, :], op=ALU.mult),
                    [pkb, "sinb"], ["r2"])
                dve(lambda e, dst=dst: e.tensor_tensor(out=dst, in0=r1[64:96, :], in1=r2[64:96, :], op=ALU.add),
                    ["r1", "r2"], [dkey])

            if stop == "fv":
                raise _Stop()
            act(sqq[:], cq[:], AF.Square, ["cq0", "cq1"], ["sqq"])
            for j in range(2):
                mm(ps[2][:], onesb[:], sqq[:, j, :], j == 0, j == 1, ["onesb", "sqq"], ["ps2"])
            rstd_from(ps[2][:], "ps2", rsq[:], "rsq", 256)
            for j in range(2):
                dve(lambda e, j=j: e.scalar_tensor_tensor(out=qn[:, j, :], in0=cq[:, j, :],
                                                          scalar=colsb[:, c0 + 56 + j:c0 + 57 + j], in1=rsq[:],
                                                          op0=ALU.mult, op1=ALU.mult),
                    ["cq%d" % j, "rsq", "cols"], ["qn"])
            for h in range(NH):
                pa, pbb = (4, 5) if h % 2 == 0 else (6, 7)
                for j in range(2):
                    mm(ps[pa][0:96, :], wqa[:, j, h * 96:(h + 1) * 96], qn[:, j, :], j == 0, j == 1, ["wqa", "qn"],
                       ["ps%d" % pa])
                for j in range(2):
                    mm(ps[pbb][0:96, :], wqb[:, j, h * 96:(h + 1) * 96], qn[:, j, :], j == 0, j == 1, ["wqb", "qn"],
                       ["ps%d" % pbb])
                act(qt[0:64, h, :], ps[pa][0:64, :], AF.Copy, ["ps%d" % pa], ["qt.n"])
                rope_rows(ps[pa], ps[pbb], "ps%d" % pa, "ps%d" % pbb, qt[64:96, h, :], "qt.r")
            dma("sp", QM_d[:, :, cs].rearrange("h r t -> r h t"), qt[:], "qt", reads=["qt.n", "qt.r"], writes=["QM.%d" % c])
            if stop == "qpath":
                raise _Stop()
            act(sqk[:], ckv[:], AF.Square, ["ckv"], ["sqk"])
            mm(ps[2][:], onesb[:], sqk[:], True, True, ["onesb", "sqk"], ["ps2"])
            rstd_from(ps[2][:], "ps2", rsk[:], "rsk", 128)
            dve(lambda e: e.scalar_tensor_tensor(out=kvn[:], in0=ckv[:], scalar=colsb[:, c0 + 58:c0 + 59], in1=rsk[:],
                                                 op0=ALU.mult, op1=ALU.mult), ["ckv", "rsk", "cols"], ["kvn"])
            for h in range(NH):
                pb = 3 + (h % 2)
                mm(ps[pb][0:96, :], wkv[:, h * 64:h * 64 + 96], kvn[:], True, True, ["wkv", "kvn"], ["ps%d" % pb])
                act(kt_[0:64, h, :], ps[pb][0:64, :], AF.Copy, ["ps%d" % pb], ["kt.n"])
                P.add("pool", lambda e, h=h: e.tensor_copy(out=kt_[64:96, h, :], in_=krope[64:96, :]),
                      reads=["krope"], writes=["kt.r"])
            dma("sp", KM_d[:, :, cs].rearrange("h r t -> r h t"), kt_[:], "kt", reads=["kt.n", "kt.r"], writes=["KM.%d" % c])
            for tt in range(4):
                pb = 4 + (tt % 2)
                mm(ps[pb][:], kvn[:, tt * 128:(tt + 1) * 128], wkv[:, 512:1024], True, True, ["kvn", "wkv"],
                   ["ps%d" % pb])
                dve(lambda e, tt=tt, pb=pb: e.tensor_copy(out=vm[:, :, tt, 0:64],
                                                          in_=ps[pb][:].rearrange("p (h d) -> p h d", h=NH)),
                    ["ps%d" % pb], ["vm"])
            dma("sp", VM_d[:, :, c * 4:(c + 1) * 4, :].rearrange("h p t d -> p h t d"), vm[:], "vm", reads=["vm"],
                writes=["VM.%d" % c])
        p1_front(0)
        for c in range(NCH):
            p1_mid(c)
            if c + 1 < NCH:
                p1_front(c + 1)
            p1_tail(c)
        if stop == "p1":
            raise _Stop()
        P.barrier()
        A.release(m1)

        m2 = A.mark()
        Kb = [A.alloc("Kb%d" % i, [96, T], BF16) for i in range(2)]
        Qb = [A.alloc("Qb%d" % i, [96, T], BF16) for i in range(2)]
        Vb = [A.alloc("Vb%d" % i, [128, NT, 65], BF16) for i in range(2)]
        Ob = [A.alloc("Ob%d" % i, [64, T], BF16) for i in range(2)]
        NPT = 5
        Pt = [A.alloc("Pt%d" % i, [128, 512], BF16) for i in range(NPT)]
        Osb = [A.alloc("Osb%d" % i, [65, 512], F32) for i in range(2)]
        ALLC = lambda pre: [pre + ".%d" % c for c in range(NCH)]
        LA = 3
        items = []
        hh = 0
        gcount = 0
        for typ in range(2):
            for h in range(NH):
                for qc in range(NCH):
                    nk = 4 * (qc + 1)
                    for kt in range(nk):
                        items.append(dict(typ=typ, h=h, hh=hh, qc=qc, kt=kt, nk=nk, g=gcount))
                    gcount += 1
                hh += 1
        loaded = set()

        def load_head(it):
            if it["hh"] in loaded:
                return
            loaded.add(it["hh"])
            typ, h, b = it["typ"], it["h"], it["hh"] % 2
            kk, qk, vk = "Kb%d" % b, "Qb%d" % b, "Vb%d" % b
            if typ == 0:
                dma("sp", Kb[b][0:96, :], KM_d[h], kk, reads=ALLC("KM"), writes=[kk + "m", kk + "a"])
                dma("sp", Qb[b][0:96, :], QM_d[h], qk, reads=ALLC("QM"), writes=[qk + "m", qk + "a"])
                dma("sp", Vb[b][:], VM_d[h], vk, reads=ALLC("VM"), writes=[vk])
            else:
                hp, ho = h // 2, (h % 2) * 64
                dma("sp", Kb[b][0:64, :], KF_d[hp, ho:ho + 64, :], kk, reads=ALLC("KF"), writes=[kk + "m"])
                dma("sp", Kb[b][64:70, :], KA_d[h], kk + "a", reads=ALLC("KA"), writes=[kk + "a"])
                dma("sp", Qb[b][0:64, :], QF_d[hp, ho:ho + 64, :], qk, reads=ALLC("QF"), writes=[qk + "m"])
                dma("sp", Qb[b][64:70, :], QA_d[h], qk + "a", reads=ALLC("QA"), writes=[qk + "a"])
                dma("sp", Vb[b][:], VF_d[h], vk, reads=ALLC("VF"), writes=[vk])

        def emit_qk(n, it):
            typ, b, qc, kt = it["typ"], it["hh"] % 2, it["qc"], it["kt"]
            R = 96 if typ == 0 else 70
            j = kt - 4 * qc
            sb, pb = n % 4, n % NPT
            skey = "ps%d" % sb
            kr = ["Kb%dm" % b, "Kb%da" % b]
            qr = ["Qb%dm" % b, "Qb%da" % b]
            c0_ = max(j, 0) * 128
            mm(ps[sb][:, c0_:512], Kb[b][0:R, kt * 128:(kt + 1) * 128], Qb[b][0:R, qc * 512 + c0_:(qc + 1) * 512],
               True, j < 0, kr + qr, [skey])
            if j >= 0:
                mm(ps[sb][:, c0_:c0_ + 128], identb[:], maskb[:, 0, 0:128], False, True, ["identb", "maskb"], [skey])
            act(Pt[pb][:, c0_:512], ps[sb][:, c0_:512], AF.Exp, [skey], ["Pt%d" % pb],
                scale=(MLA_SCALE if typ == 0 else 1.0))

        def emit_pv(n, it):
            b, kt, nk, ob = it["hh"] % 2, it["kt"], it["nk"], it["g"] % 2
            pb = n % NPT
            c0_ = max(kt - 4 * it["qc"], 0) * 128
            mm(ps[4 + ob][0:65, c0_:512], Vb[b][:, kt, :], Pt[pb][:, c0_:512], kt == 0, kt == nk - 1,
               ["Vb%d" % b, "Pt%d" % pb], ["ps%d" % (4 + ob)])

        def epi1(it):
            ob = it["g"] % 2
            while deferred and deferred[0][1]["g"] <= it["g"] - 2:
                epi2(deferred.pop(0)[1])
            dve(lambda e: e.tensor_copy(out=Osb[ob][:], in_=ps[4 + ob][0:65, :]), ["ps%d" % (4 + ob)], ["Osb%d" % ob])
            act(Osb[ob][64:65, :], Osb[ob][64:65, :], AF.Ln, ["Osb%d" % ob], ["Osb%d" % ob])
            act(Osb[ob][64:65, :], Osb[ob][64:65, :], AF.Exp, ["Osb%d" % ob], ["Osb%d" % ob], scale=-1.0)

        def epi2(it):
            ob, b, qc, typ, h = it["g"] % 2, it["hh"] % 2, it["qc"], it["typ"], it["h"]
            ok = "Ob%d" % b
            mm(ps[6 + ob][0:96, :], sell[0:65, :], Osb[ob][:], True, True, ["sell", "Osb%d" % ob], ["ps%d" % (6 + ob)])
            dve(lambda e: e.tensor_tensor(out=Ob[b][:, qc * 512:(qc + 1) * 512], in0=Osb[ob][0:64, :],
                                          in1=ps[6 + ob][0:64, :], op=ALU.mult),
                ["Osb%d" % ob, "ps%d" % (6 + ob)], [ok])
            if qc == NCH - 1:
                kch = typ * 4 + h // 2
                ho = (h % 2) * 64
                dma("sp", OT_d[kch, ho:ho + 64, :], Ob[b][:], ok, reads=[ok], writes=["OT.%d.%d" % (kch, h % 2)])

        NI = len(items)
        deferred = []
        for n in range(NI + LA):
            if n < NI:
                load_head(items[n])
                emit_qk(n, items[n])
            m_ = n - LA
            if m_ >= 0:
                it = items[m_]
                if it["kt"] == 0 and it["qc"] == 0:
                    nxt = [x for x in items[m_:m_ + 40 * NCH * NCH + 8] if x["hh"] == it["hh"] + 1]
                    if nxt:
                        load_head(nxt[0])
                emit_pv(m_, it)
                if it["kt"] == it["nk"] - 1:
                    epi1(it)
                    deferred.append((n + 6, it))
            while deferred and (deferred[0][0] <= n or n == NI + LA - 1):
                epi2(deferred.pop(0)[1])
        if stop == "p2":
            raise _Stop()
        P.barrier()
        A.release(m2)

        m3 = A.mark()
        wo = A.alloc("wo", [128, KC, D], BF16)
        dma("pool", wo[:], wo_d[l].rearrange("(k p) n -> p k n", p=128), "wo", writes=["wo"])
        ot = [A.alloc("ot%d" % i_, [128, KC, 512], BF16) for i_ in range(2)]
        sqo = [A.alloc("sqo%d" % i_, [128, KC, 512], BF16) for i_ in range(2)]
        on = [A.alloc("on%d" % i_, [128, KC, 512], BF16) for i_ in range(2)]
        xt = [A.alloc("xt3%d" % i_, [128, KC, 512], F32) for i_ in range(2)]
        tmp = A.alloc("tmp3", [128, 2, 512], F32)
        h2 = [A.alloc("h2%d" % i_, [128, KC, 512], BF16) for i_ in range(2)]
        rsm = A.alloc("rsm", [128, 512], F32)
        rsf = A.alloc("rsf", [128, 512], F32)
        rs2 = A.alloc("rs2", [128, 512], F32)
        if is_moe:
            mi = moe_layers.index(l)
            h2f = [A.alloc("h2f%d" % i_, [128, KC, 512], F32) for i_ in range(2)]
            rw = A.alloc("rw", [128, KC, NE], F32)
            dma("sp", rw[:], rw_d[mi].rearrange("(k p) e -> p k e", p=128), "rw", writes=["rw"])
            lg = A.alloc("lg", [128, 4, NE], F32)
            m1t = A.alloc("m1t", [128, 4], F32)
            nm1 = A.alloc("nm1", [128, 4], F32)
            m2t = A.alloc("m2t", [128, 4], F32)
            eq = A.alloc("eq", [128, 4, NE], F32)
            mk = A.alloc("mk", [128, 4, NE], F32)
            selt = A.alloc("selt", [128, 4, NE], F32)
            ex = A.alloc("ex", [128, 4, NE], F32)
            den = A.alloc("den", [128, 4], F32)
            gt = A.alloc("gt", [128, 4, 128], F32)
            gT = A.alloc("gT", [128, 512], F32)
            gb = A.alloc("gb", [128, NE, 512], F32)
            dve(lambda e: e.memset(gt[:], 0.0), [], ["gt"])
        ALLO = ["OT.%d.%d" % (k, s) for k in range(KC) for s in range(2)]
        for c in range(NCH):
            p_ = c % 2
            cs = slice(c * 512, (c + 1) * 512)
            def p3_loads(cc):
                q_ = cc % 2
                cs_ = slice(cc * 512, (cc + 1) * 512)
                dma("sp", ot[q_][:], OT_d[:, :, cs_].rearrange("k p t -> p k t"), "ot%d" % q_, reads=ALLO,
                    writes=["ot%d" % q_])
                dma("sp", xt[q_][:], xT_d[:, :, cs_].rearrange("k p t -> p k t"), "xt3%d" % q_, reads=["xT.%d" % cc],
                    writes=["xt3%d" % q_])
            if c == 0:
                p3_loads(0)
            if c + 1 < NCH:
                p3_loads(c + 1)
            if stop == "p3a":
                raise _Stop()
            act(sqo[p_][:], ot[p_][:], AF.Square, ["ot%d" % p_], ["sqo%d" % p_])
            for k in range(4):
                mm(ps[0][:], onesb[:], sqo[p_][:, k, :], k == 0, k == 3, ["onesb", "sqo%d" % p_], ["ps0"])
            for k in range(4):
                mm(ps[1][:], onesb[:], sqo[p_][:, 4 + k, :], k == 0, k == 3, ["onesb", "sqo%d" % p_], ["ps1"])
            if stop == "p3b":
                raise _Stop()
            rstd_from(ps[0][:], "ps0", rsm[:], "rsm", 512)
            rstd_from(ps[1][:], "ps1", rsf[:], "rsf", 512)
            for k in range(KC):
                rsx, rkey = (rsm, "rsm") if k < 4 else (rsf, "rsf")
                gcol = c0 + 59 + k
                dve(lambda e, k=k, rsx=rsx, gcol=gcol: e.scalar_tensor_tensor(
                    out=on[p_][:, k, :], in0=ot[p_][:, k, :], scalar=colsb[:, gcol:gcol + 1], in1=rsx[:], op0=ALU.mult,
                    op1=ALU.mult), ["ot%d" % p_, rkey, "cols"], ["on%d_%d" % (p_, k)])
            if stop == "p3c":
                raise _Stop()
            for i in range(KC):
                pb = 2 + (i % 2)
                for k in range(KC):
                    mm(ps[pb][:], wo[:, k, i * 128:(i + 1) * 128], on[p_][:, k, :], k == 0, k == KC - 1,
                       ["wo", "on%d_%d" % (p_, k)], ["ps%d" % pb])
                dve(lambda e, i=i, pb=pb: e.scalar_tensor_tensor(out=xt[p_][:, i, :], in0=ps[pb][:],
                                                                 scalar=modsb[l][:, 16 + i:17 + i], in1=xt[p_][:, i, :],
                                                                 op0=ALU.mult, op1=ALU.add),
                    ["ps%d" % pb, "mod%d" % l, "xt3%d" % p_], ["xt3%d" % p_])
            dma("sp", xT_d[:, :, cs].rearrange("k p t -> p k t"), xt[p_][:], "xt3%d" % p_, reads=["xt3%d" % p_], writes=["xT.%d" % c])
            if stop == "p3e":
                raise _Stop()
            act(sqo[p_][:], xt[p_][:], AF.Square, ["xt3%d" % p_], ["sqo%d" % p_])
            for k in range(KC):
                mm(ps[4][:], onesb[:], sqo[p_][:, k, :], k == 0, k == KC - 1, ["onesb", "sqo%d" % p_], ["ps4"])
            rstd_from(ps[4][:], "ps4", rs2[:], "rs2", D)
            for k in range(KC):
                dve(lambda e, k=k: e.tensor_tensor(out=tmp[:, k % 2, :], in0=xt[p_][:, k, :], in1=rs2[:], op=ALU.mult),
                    ["xt3%d" % p_, "rs2"], ["tmp3%d" % (k % 2)])
                if is_moe:
                    act(h2f[p_][:, k, :], tmp[:, k % 2, :], AF.Identity, ["tmp3%d" % (k % 2), "gsf%d" % l, "mod%d" % l],
                        ["h2f%d_%d" % (p_, k)], bias=modsb[l][:, 24 + k:25 + k], scale=gsf[l][:, k:k + 1])
                    P.add("pool", lambda e, k=k: e.tensor_copy(out=h2[p_][:, k, :], in_=h2f[p_][:, k, :]),
                          reads=["h2f%d_%d" % (p_, k)], writes=["h2_%d" % p_])
                else:
                    act(h2[p_][:, k, :], tmp[:, k % 2, :], AF.Identity, ["tmp3%d" % (k % 2), "gsf%d" % l, "mod%d" % l], ["h2_%d" % p_],
                        bias=modsb[l][:, 24 + k:25 + k], scale=gsf[l][:, k:k + 1])
            dma("sp", H2_d[:, :, cs].rearrange("k p t -> p k t"), h2[p_][:], "h2_%d" % p_, reads=["h2_%d" % p_], writes=["H2.%d" % c])
            if is_moe:
                for tt in range(4):
                    for k in range(KC):
                        mm(ps[5][:, tt * NE:(tt + 1) * NE], h2f[p_][:, k, tt * 128:(tt + 1) * 128], rw[:, k, :], k == 0,
                           k == KC - 1, ["h2f%d_%d" % (p_, k), "rw"], ["ps5"])
                dve(lambda e: e.tensor_copy(out=lg[:], in_=ps[5][:, 0:4 * NE].rearrange("p (t e) -> p t e", t=4)),
                    ["ps5"], ["lg"])
                dve(lambda e: e.tensor_reduce(out=m1t[:], in_=lg[:], axis=AX.X, op=ALU.max), ["lg"], ["m1t"])
                dve(lambda e: e.tensor_scalar(out=nm1[:], in0=m1t[:], scalar1=-1.0, scalar2=None, op0=ALU.mult),
                    ["m1t"], ["nm1"])
                for tt in range(4):
                    dve(lambda e, tt=tt: e.tensor_scalar(out=eq[:, tt, :], in0=lg[:, tt, :], scalar1=m1t[:, tt:tt + 1],
                                                         scalar2=None, op0=ALU.is_equal), ["lg", "m1t"], ["eq"])
                dve(lambda e: e.scalar_tensor_tensor(out=mk[:], in0=eq[:], scalar=-1e30, in1=lg[:], op0=ALU.mult,
                                                     op1=ALU.add), ["eq", "lg"], ["mk"])
                dve(lambda e: e.tensor_reduce(out=m2t[:], in_=mk[:], axis=AX.X, op=ALU.max), ["mk"], ["m2t"])
                for tt in range(4):
                    dve(lambda e, tt=tt: e.tensor_scalar(out=selt[:, tt, :], in0=lg[:, tt, :], scalar1=m2t[:, tt:tt + 1],
                                                         scalar2=None, op0=ALU.is_ge), ["lg", "m2t"], ["selt"])
                    act(ex[:, tt, :], lg[:, tt, :], AF.Exp, ["lg", "nm1"], ["ex"], bias=nm1[:, tt:tt + 1], scale=1.0)
                    act(den[:, tt:tt + 1], m2t[:, tt:tt + 1], AF.Exp, ["m2t", "nm1"], ["den"], bias=nm1[:, tt:tt + 1],
                        scale=1.0)
                dve(lambda e: e.tensor_scalar(out=den[:], in0=den[:], scalar1=1.0, scalar2=None, op0=ALU.add),
                    ["den"], ["den"])
                dve(lambda e: e.reciprocal(out=den[:], in_=den[:]), ["den"], ["den"])
                for tt in range(4):
                    dve(lambda e, tt=tt: e.scalar_tensor_tensor(out=gt[:, tt, 0:NE], in0=ex[:, tt, :],
                                                                scalar=den[:, tt:tt + 1], in1=selt[:, tt, :],
                                                                op0=ALU.mult, op1=ALU.mult),
                        ["ex", "den", "selt"], ["gt"])
                for tt in range(4):
                    mm(ps[6][:, tt * 128:(tt + 1) * 128], gt[:, tt, :], identf[:], True, True, ["gt", "identf"], ["ps6"])
                dve(lambda e: e.tensor_copy(out=gT[:], in_=ps[6][:]), ["ps6"], ["gT"])
                for ex_ in range(NE):
                    pb = 6 + (ex_ % 2)
                    mm(ps[pb][:], sele[:, ex_, :], gT[:], True, True, ["sele", "gT"], ["ps%d" % pb])
                    act(gb[:, ex_, :], ps[pb][:], AF.Copy, ["ps%d" % pb], ["gb"])
                dma("sp", GB_d[:, :, cs].rearrange("e p t -> p e t"), gb[:], "gb", reads=["gb"], writes=["GB.%d" % c])
        if stop == "p3":
            raise _Stop()
        P.barrier()
        A.release(m3)

        m4 = A.mark()
        TB = min(T, 2048)
        NSB = TB // 512
        h2b = A.alloc("h2b", [128, KC, TB], BF16)
        yacc = A.alloc("yacc", [128, KC, TB], F32)
        wg = [A.alloc("wg%d" % i, [128, KC, 512], BF16) for i in range(2)]
        wu = [A.alloc("wu%d" % i, [128, KC, 512], BF16) for i in range(2)]
        wd = [A.alloc("wd%d" % i, [128, 4, D], BF16) for i in range(2)]
        actb = [A.alloc("actb%d" % i, [128, 4, 512], BF16) for i in range(2)]
        sg = [A.alloc("sg%d" % i, [128, 512], F32) for i in range(2)]
        ta = [A.alloc("ta%d" % i, [128, 512], F32) for i in range(2)] if is_moe else None
        gbe = [A.alloc("gbe%d" % i, [128, TB], F32) for i in range(2)] if is_moe else None
        xt = A.alloc("xt4", [128, KC, 512], F32)
        nexp = NE if is_moe else 1
        wcount = 0
        gcount = 0
        for blk in range(T // TB):
            bs = slice(blk * TB, (blk + 1) * TB)
            chunks = range(blk * NSB, (blk + 1) * NSB)
            dma("sp", h2b[:], H2_d[:, :, bs].rearrange("k p t -> p k t"), "h2b",
                reads=["H2.%d" % c for c in chunks], writes=["h2b"])
            for ei in range(nexp):
                if is_moe:
                    mi = moe_layers.index(l)
                    Wg, Wu, Wd = mg_d[mi, ei], mu_d[mi, ei], md_d[mi, ei]
                    gbb = gcount % 2
                    gcount += 1
                    dma("sp", gbe[gbb][:], GB_d[ei, :, bs], "gbe%d" % gbb, reads=["GB.%d" % c for c in chunks],
                        writes=["gbe%d" % gbb])
                else:
                    di = [x for x in range(depth) if x not in moe_layers].index(l)
                    Wg, Wu, Wd = dg_d[di], du_d[di], dd_d[di]
                for fg in range(NFG):
                    wb = wcount % 2
                    wcount += 1
                    fs = slice(fg * 512, (fg + 1) * 512)
                    dma("pool", wg[wb][:], Wg[:, fs].rearrange("(k p) n -> p k n", p=128), "wg%d" % wb,
                        writes=["wg%d" % wb])
                    dma("pool", wu[wb][:], Wu[:, fs].rearrange("(k p) n -> p k n", p=128), "wu%d" % wb,
                        writes=["wu%d" % wb])
                    dma("pool", wd[wb][:], Wd[fs, :].rearrange("(j p) n -> p j n", p=128), "wd%d" % wb,
                        writes=["wd%d" % wb])
                    for sbi in range(NSB):
                        ss = slice(sbi * 512, (sbi + 1) * 512)
                        ab = (wcount * NSB + sbi) % 2
                        akey = "actb%d" % ab
                        for jj in range(4):
                            g_i = jj % 2
                            pg, pu = g_i, 2 + g_i
                            for k in range(KC):
                                mm(ps[pg][:], wg[wb][:, k, jj * 128:(jj + 1) * 128], h2b[:, k, ss], k == 0, k == KC - 1,
                                   ["wg%d" % wb, "h2b"], ["ps%d" % pg])
                            for k in range(KC):
                                mm(ps[pu][:], wu[wb][:, k, jj * 128:(jj + 1) * 128], h2b[:, k, ss], k == 0, k == KC - 1,
                                   ["wu%d" % wb, "h2b"], ["ps%d" % pu])
                            act(sg[g_i][:], ps[pg][:], AF.Silu, ["ps%d" % pg], ["sg%d" % g_i])
                            if is_moe:
                                dve(lambda e, g_i=g_i, pu=pu: e.tensor_tensor(out=ta[g_i][:], in0=sg[g_i][:],
                                                                              in1=ps[pu][:], op=ALU.mult),
                                    ["sg%d" % g_i, "ps%d" % pu], ["ta%d" % g_i])
                                dve(lambda e, g_i=g_i, ab=ab, jj=jj, gbb=gbb, ss=ss: e.tensor_tensor(
                                    out=actb[ab][:, jj, :], in0=ta[g_i][:], in1=gbe[gbb][:, ss], op=ALU.mult),
                                    ["ta%d" % g_i, "gbe%d" % gbb], [akey + ".%d" % jj])
                            else:
                                dve(lambda e, g_i=g_i, pu=pu, ab=ab, jj=jj: e.tensor_tensor(
                                    out=actb[ab][:, jj, :], in0=sg[g_i][:], in1=ps[pu][:], op=ALU.mult),
                                    ["sg%d" % g_i, "ps%d" % pu], [akey + ".%d" % jj])
                        first = (ei == 0 and fg == 0)
                        for i in range(KC):
                            py = 4 + (i % 2)
                            for jj in range(4):
                                mm(ps[py][:], wd[wb][:, jj, i * 128:(i + 1) * 128], actb[ab][:, jj, :], jj == 0, jj == 3,
                                   ["wd%d" % wb, akey + ".%d" % jj], ["ps%d" % py])
                            ykey = "yacc.%d.%d" % (i, sbi)
                            if first:
                                act(yacc[:, i, ss], ps[py][:], AF.Copy, ["ps%d" % py], [ykey])
                            else:
                                dve(lambda e, i=i, py=py, ss=ss: e.tensor_tensor(out=yacc[:, i, ss], in0=ps[py][:],
                                                                                 in1=yacc[:, i, ss], op=ALU.add),
                                    ["ps%d" % py, ykey], [ykey])
            for sbi in range(NSB):
                c = blk * NSB + sbi
                cs = slice(c * 512, (c + 1) * 512)
                ss = slice(sbi * 512, (sbi + 1) * 512)
                dma("sp", xt[:], xT_d[:, :, cs].rearrange("k p t -> p k t"), "xt4", reads=["xT.%d" % c], writes=["xt4"])
                for i in range(KC):
                    dve(lambda e, i=i, ss=ss: e.scalar_tensor_tensor(out=xt[:, i, :], in0=yacc[:, i, ss],
                                                                     scalar=modsb[l][:, 40 + i:41 + i], in1=xt[:, i, :],
                                                                     op0=ALU.mult, op1=ALU.add),
                        ["yacc.%d.%d" % (i, sbi), "mod%d" % l, "xt4"], ["xt4"])
                dma("sp", xT_d[:, :, cs].rearrange("k p t -> p k t"), xt[:], "xt4", reads=["xt4"], writes=["xT.%d" % c])
        if stop == "p4":
            raise _Stop()
        P.barrier()
        A.release(m4)

    xt = [A.alloc("xt5%d" % i_, [128, KC, 512], F32) for i_ in range(2)]
    sq = A.alloc("sq5", [128, KC, 512], BF16)
    rs = A.alloc("rs5", [128, 512], F32)
    yT = A.alloc("yT", [128, KC, 512], F32)
    yo = [A.alloc("yo%d" % i, [128, D], F32) for i in range(2)]
    ocount = 0
    for c in range(NCH):
        cs = slice(c * 512, (c + 1) * 512)
        def p5_load(cc):
            dma("sp", xt[cc % 2][:], xT_d[:, :, cc * 512:(cc + 1) * 512].rearrange("k p t -> p k t"), "xt5%d" % (cc % 2),
                reads=["xT.%d" % cc], writes=["xt5%d" % (cc % 2)])
        if c == 0:
            p5_load(0)
        if c + 1 < NCH:
            p5_load(c + 1)
        p_ = c % 2
        act(sq[:], xt[p_][:], AF.Square, ["xt5%d" % p_], ["sq5"])
        for k in range(KC):
            mm(ps[0][:], onesb[:], sq[:, k, :], k == 0, k == KC - 1, ["onesb", "sq5"], ["ps0"])
        rstd_from(ps[0][:], "ps0", rs[:], "rs5", D)
        for k in range(KC):
            dve(lambda e, k=k: e.scalar_tensor_tensor(out=yT[:, k, :], in0=xt[p_][:, k, :], scalar=colsb[:, 150 + k:151 + k],
                                                      in1=rs[:], op0=ALU.mult, op1=ALU.mult),
                ["xt5%d" % p_, "rs5", "cols"], ["yT"])
        for tt in range(4):
            ob = ocount % 2
            ocount += 1
            for half in range(2):
                pb = 2 + 2 * (tt % 2) + half
                for kq in range(4):
                    k = half * 4 + kq
                    mm(ps[pb][:, kq * 128:(kq + 1) * 128], yT[:, k, tt * 128:(tt + 1) * 128], identf[:], True, True,
                       ["yT", "identf"], ["ps%d" % pb])
                if half == 0:
                    act(yo[ob][:, 0:512], ps[pb][:], AF.Copy, ["ps%d" % pb], ["yo%d" % ob])
                else:
                    dve(lambda e, ob=ob, pb=pb: e.tensor_copy(out=yo[ob][:, 512:1024], in_=ps[pb][:]), ["ps%d" % pb],
                        ["yo%d" % ob])
            r0 = c * 512 + tt * 128
            dma("sp", out_d[r0:r0 + 128, :], yo[ob][:], "yo%d" % ob, reads=["yo%d" % ob], writes=["out.%d" % (r0 // 128)])
    P.finish()
    P.emit()
    es.close()
    return nc, es


def _col(v):
    v = np.asarray(v, dtype=np.float32)
    return np.ascontiguousarray(v.reshape(-1, 128).T)


def _const_tables():
    cf = np.zeros((128, CONSTF), np.float32)
    cf[:, 0:128] = np.eye(128, dtype=np.float32)
    cf[:, 128:256] = 1.0
    p = np.arange(128)[:, None]
    q = np.arange(512)[None, :]
    for j in range(4):
        cf[:, 256 + j * 512:256 + (j + 1) * 512] = np.where(j * 128 + p > q, -30000.0, 0.0)
    cf[64, 2304:2400] = 1.0
    for e in range(NE):
        cf[e, 2400 + e * 128:2400 + (e + 1) * 128] = 1.0
    half = 16
    inv_freq = (np.float32(10000.0) ** (-np.arange(half, dtype=np.float32) / np.float32(half))).astype(np.float32)
    rope = np.zeros((128, 2), np.float32)
    rope[:, 0] = inv_freq[np.arange(128) % 16]
    rope[:, 1] = np.where((np.arange(128) % 32) < 16, -1.0, 1.0)
    return cf, rope


def prepare_inputs(depth, moe_layers, T, x, c, positions, ada_w, ada_b, attn_norm_g, w_in, q_norm_g, w_uq, kv_norm_g,
                   w_ukv, fox_forget_b, mla_out_g, fox_out_g, w_o, ffn_norm_g, dense_w_gate, dense_w_up, dense_w_down,
                   router_w, moe_w_gate, moe_w_up, moe_w_down, final_norm_g, ncores):
    f32 = lambda a: np.ascontiguousarray(np.asarray(a, dtype=np.float32))
    cf, rope = _const_tables()
    w_in = f32(w_in)[:depth]
    w_uq, w_ukv, ada_w, w_o = f32(w_uq)[:depth], f32(w_ukv)[:depth], f32(ada_w)[:depth], f32(w_o)[:depth]
    cq_, ckv_, kr_, fq_, fk_, fv_, fl_ = (w_in[:, :, 0:256], w_in[:, :, 256:384], w_in[:, :, 384:416],
                                          w_in[:, :, 416:928], w_in[:, :, 928:1440], w_in[:, :, 1440:1952],
                                          w_in[:, :, 1952:1960])
    krb = np.concatenate([kr_[:, :, 16:32], kr_[:, :, 0:16]], axis=-1)
    w_in_p = np.ascontiguousarray(np.concatenate([cq_, ckv_, kr_, ckv_[:, :, 64:128], krb, fl_, fq_, fk_, fv_], axis=-1))
    assert w_in_p.shape[-1] == WIN_COLS
    w_uq = f32(w_uq).reshape(depth, 256, NH, 96)
    wqb = np.concatenate([w_uq[..., 0:64], w_uq[..., 80:96], w_uq[..., 64:80]], axis=-1)
    w_uqa = np.ascontiguousarray(w_uq.reshape(depth, 256, 768))
    w_uqb = np.ascontiguousarray(wqb.reshape(depth, 256, 768))
    wkv = f32(w_ukv).reshape(depth, 128, NH, 128)
    w_ukv_p = np.ascontiguousarray(np.concatenate([wkv[..., 0:64].reshape(depth, 128, 512),
                                                   wkv[..., 64:128].reshape(depth, 128, 512)], axis=-1))
    shared = {
        "constf": cf, "ada_w": f32(ada_w), "w_in": w_in_p, "w_uqa": w_uqa, "w_uqb": w_uqb, "w_ukv": w_ukv_p,
        "w_o": f32(w_o), "dense_w_gate": f32(dense_w_gate), "dense_w_up": f32(dense_w_up),
        "dense_w_down": f32(dense_w_down), "router_w": f32(router_w), "moe_w_gate": f32(moe_w_gate),
        "moe_w_up": f32(moe_w_up), "moe_w_down": f32(moe_w_down),
    }
    in_maps = []
    for b in range(ncores):
        cols = np.zeros((128, NCOL), np.float32)
        for l in range(depth):
            o = l * LCOLS
            cols[:, o:o + 8] = _col(attn_norm_g[l])
            cols[:, o + 8:o + 56] = _col(ada_b[l])
            cols[:, o + 56:o + 58] = _col(q_norm_g[l])
            cols[:, o + 58:o + 59] = _col(kv_norm_g[l])
            cols[:, o + 59:o + 63] = _col(mla_out_g[l])
            cols[:, o + 63:o + 67] = _col(fox_out_g[l])
            cols[:, o + 67:o + 75] = _col(ffn_norm_g[l])
            cols[0:8, 168 + l] = np.asarray(fox_forget_b[l], np.float32)
        cols[:, 150:158] = _col(final_norm_g)
        cols[:, 158:166] = _col(np.asarray(c)[b])
        cols[:, 166:168] = rope
        m = dict(shared)
        m["x"] = f32(np.asarray(x)[b, :T])
        m["posb"] = np.ascontiguousarray(np.broadcast_to(np.asarray(positions)[b, :T].astype(np.int32), (128, T)))
        m["cols"] = cols
        in_maps.append(m)
    return in_maps


_CACHE = {}


def run_config(T, depth, moe_layers, ncores, inputs, debug=False, stop=None):
    key = (T, depth, tuple(moe_layers), debug, stop)
    if key not in _CACHE:
        _CACHE[key] = build_program(T, depth, list(moe_layers), debug=debug, stop=stop)
    nc, _es = _CACHE[key]
    in_maps = prepare_inputs(depth, list(moe_layers), T, ncores=ncores, **inputs)
    res = run_bass_kernel_spmd(nc, in_maps, core_ids=list(range(ncores)))
    out = np.stack([np.asarray(r["out"]) for r in res.results], axis=0)
    if debug:
        return out.astype(np.float32), res.results
    return out.astype(np.float32)


def kernel(**inputs):
    return run_config(4096, 2, [1], 8, inputs)
```
